# Optimizing a Trainium2 kernel written in Bass

```python
import jax, jax.numpy as jnp
from jax import lax
import numpy as np

D_MODEL = 1024
BATCH = 16
SEQ = 4096
DEPTH = 2

CHUNK = 64
D_MIX = D_MODEL
EPS = 1e-6

RET_HEADS = 4
RET_DK = 64
RET_DV = 64
RET_WIDTH = RET_HEADS * RET_DV
ROPE_BASE = 10000.0

SSD_HEADS = 8
SSD_HEAD_DIM = 64
SSD_WIDTH = SSD_HEADS * SSD_HEAD_DIM
SSD_GROUPS = 2
SSD_HEADS_PER_GROUP = SSD_HEADS // SSD_GROUPS
SSD_STATE = 128
SSD_CONV = 4
SSD_CONV_DIM = SSD_WIDTH + 2 * SSD_GROUPS * SSD_STATE

LRU_WIDTH = D_MIX - RET_WIDTH - SSD_WIDTH
LRU_BLOCKS = 4
LRU_BLOCK_DIM = LRU_WIDTH // LRU_BLOCKS
LRU_CONV = 4
LRU_C = 8.0

D_FF = 2816
FFN_CONV = 3

PROJ_SIZES = (RET_HEADS * RET_DK, RET_HEADS * RET_DK, RET_WIDTH, RET_WIDTH,
              SSD_WIDTH, SSD_CONV_DIM, SSD_HEADS,
              LRU_WIDTH, LRU_WIDTH)
D_PROJ = sum(PROJ_SIZES)

kernel_name = 'hybrid_retention_ssd_rglru_convffn'


def rmsnorm(x, w):
    xf = x.astype(jnp.float32)
    y = xf * lax.rsqrt(jnp.mean(xf * xf, axis=-1, keepdims=True) + EPS)
    return (y * w.astype(jnp.float32)).astype(x.dtype)


def causal_dwconv(x, w, b):
    width = w.shape[0]
    y = lax.conv_general_dilated(x, w[:, None, :].astype(x.dtype), window_strides=(1,),
                                 padding=[(width - 1, 0)],
                                 dimension_numbers=('NWC', 'WIO', 'NWC'),
                                 feature_group_count=x.shape[-1])
    return y + b.astype(x.dtype)


def split_proj(p):
    pieces = []
    start = 0
    for size in PROJ_SIZES:
        pieces.append(p[..., start:start + size])
        start += size
    return pieces


def rotary(x, pos):
    half = x.shape[-1] // 2
    inv = ROPE_BASE ** (-jnp.arange(half, dtype=jnp.float32) / half)
    ang = pos[:, None] * inv[None, :]
    cos = jnp.cos(ang)[None, :, None, :]
    sin = jnp.sin(ang)[None, :, None, :]
    x1, x2 = x[..., :half], x[..., half:]
    return jnp.concatenate([x1 * cos - x2 * sin, x1 * sin + x2 * cos], axis=-1)


def retention(q, k, v, g):
    b, L = q.shape[0], q.shape[1]
    nc = L // CHUNK
    pos = jnp.arange(L, dtype=jnp.float32)
    q = rotary(q.reshape(b, L, RET_HEADS, RET_DK), pos) * (RET_DK ** -0.5)
    k = rotary(k.reshape(b, L, RET_HEADS, RET_DK), pos)
    qc = q.reshape(b, nc, CHUNK, RET_HEADS, RET_DK)
    kc = k.reshape(b, nc, CHUNK, RET_HEADS, RET_DK)
    vc = v.reshape(b, nc, CHUNK, RET_HEADS, RET_DV)
    log_gamma = jnp.log(1.0 - 2.0 ** (-5.0 - jnp.arange(RET_HEADS, dtype=jnp.float32)))
    idx = jnp.arange(CHUNK, dtype=jnp.float32)
    diff = idx[:, None] - idx[None, :]
    dmask = jnp.where(diff >= 0, jnp.exp(log_gamma[:, None, None] * jnp.maximum(diff, 0.0)), 0.0)
    scores = jnp.einsum('bclhd,bcshd->bhcls', qc, kc) * dmask[:, None]
    y_intra = jnp.einsum('bhcls,bcshe->bclhe', scores, vc)
    k_decay = jnp.exp(log_gamma[:, None] * (CHUNK - 1 - idx)[None, :])
    kv = jnp.einsum('bcshd,hs,bcshe->cbhde', kc, k_decay, vc)
    chunk_decay = jnp.exp(log_gamma * CHUNK)[:, None, None]

    def step(state, kv_c):
        return chunk_decay * state + kv_c, state

    _, s_prev = lax.scan(step, jnp.zeros(kv.shape[1:], kv.dtype), kv)
    q_decay = jnp.exp(log_gamma[:, None] * (idx + 1.0)[None, :])
    y_inter = jnp.einsum('bclhd,hl,cbhde->bclhe', qc, q_decay, s_prev)
    y = (y_intra + y_inter).reshape(b, L, RET_HEADS, RET_DV).astype(jnp.float32)
    mu = jnp.mean(y, axis=-1, keepdims=True)
    var = jnp.mean(jnp.square(y - mu), axis=-1, keepdims=True)
    y = ((y - mu) * lax.rsqrt(var + EPS)).reshape(b, L, RET_WIDTH)
    return y.astype(q.dtype) * jax.nn.silu(g)


def ssd(z, xbc, dt_raw, conv_w, conv_b, dt_bias, a_log, d_skip, norm_w):
    b, L = z.shape[0], z.shape[1]
    nc = L // CHUNK
    G, R, P, N = SSD_GROUPS, SSD_HEADS_PER_GROUP, SSD_HEAD_DIM, SSD_STATE
    xbc = jax.nn.silu(causal_dwconv(xbc, conv_w, conv_b))
    xs = xbc[..., :SSD_WIDTH]
    bm = xbc[..., SSD_WIDTH:SSD_WIDTH + G * N]
    cm = xbc[..., SSD_WIDTH + G * N:]
    dt = jax.nn.softplus(dt_raw.astype(jnp.float32) + dt_bias.astype(jnp.float32))
    a = -jnp.exp(a_log.astype(jnp.float32))
    xc = xs.reshape(b, nc, CHUNK, G, R, P)
    bc = bm.reshape(b, nc, CHUNK, G, N)
    cc = cm.reshape(b, nc, CHUNK, G, N)
    dtc = dt.reshape(b, nc, CHUNK, G, R)
    cum = jnp.cumsum((dtc * a.reshape(G, R)).transpose(0, 3, 4, 1, 2), axis=-1)
    tril = jnp.tril(jnp.ones((CHUNK, CHUNK), dtype=bool))
    seg = cum[..., :, None] - cum[..., None, :]
    lmat = jnp.where(tril, jnp.exp(jnp.where(tril, seg, 0.0)), 0.0)
    xdt = xc * dtc[..., None]
    cb = jnp.einsum('bclgn,bcsgn->bgcls', cc, bc)
    y_diag = jnp.einsum('bgcls,bgrcls,bcsgrp->bclgrp', cb, lmat, xdt)
    decay_states = jnp.exp(cum[..., -1:] - cum)
    states = jnp.einsum('bcsgn,bgrcs,bcsgrp->cbgrpn', bc, decay_states, xdt)
    chunk_decay = jnp.exp(cum[..., -1]).transpose(3, 0, 1, 2)

    def step(state, inp):
        dec, st = inp
        return dec[..., None, None] * state + st, state

    _, s_prev = lax.scan(step, jnp.zeros(states.shape[1:], states.dtype), (chunk_decay, states))
    y_off = jnp.einsum('bclgn,cbgrpn,bgrcl->bclgrp', cc, s_prev, jnp.exp(cum))
    y = y_diag + y_off + d_skip.reshape(G, R)[:, :, None] * xc
    y = y.reshape(b, L, SSD_WIDTH) * jax.nn.silu(z)
    yf = y.astype(jnp.float32).reshape(b, L, G, SSD_WIDTH // G)
    yf = yf * lax.rsqrt(jnp.mean(yf * yf, axis=-1, keepdims=True) + EPS)
    return (yf.reshape(b, L, SSD_WIDTH) * norm_w.astype(jnp.float32)).astype(z.dtype)


def rglru(gate, xin, conv_w, conv_b, w_a, b_a, w_x, b_x, lam):
    b, L = xin.shape[0], xin.shape[1]
    xc = causal_dwconv(xin, conv_w, conv_b)
    xb = xc.reshape(b, L, LRU_BLOCKS, LRU_BLOCK_DIM)
    r = jax.nn.sigmoid(jnp.einsum('blki,kij->blkj', xb, w_a).reshape(b, L, LRU_WIDTH) + b_a)
    i = jax.nn.sigmoid(jnp.einsum('blki,kij->blkj', xb, w_x).reshape(b, L, LRU_WIDTH) + b_x)
    log_a = -LRU_C * r.astype(jnp.float32) * jax.nn.softplus(-lam.astype(jnp.float32))
    a = jnp.exp(log_a)
    u = jnp.sqrt(-jnp.expm1(2.0 * log_a)) * (i * xc)

    def combine(lhs, rhs):
        a1, b1 = lhs
        a2, b2 = rhs
        return a1 * a2, a2 * b1 + b2

    _, h = lax.associative_scan(combine, (a, u), axis=1)
    return h.astype(xin.dtype) * jax.nn.gelu(gate)


def conv_ffn(x, w_up, conv_w, conv_b, w_down):
    h = causal_dwconv(x @ w_up, conv_w, conv_b)
    u, v = h[..., :D_FF], h[..., D_FF:]
    return (jax.nn.gelu(u) * v) @ w_down


def setup_inputs(seed: int = 0) -> dict:
    key = jax.random.key(seed)
    ks = jax.random.split(key, 24)
    f32 = jnp.float32

    def nrm(k, shape, scale):
        return jax.random.normal(k, shape, f32) * scale

    x = jax.random.normal(ks[0], (BATCH, SEQ, D_MODEL), f32)
    norm1_w = 1.0 + nrm(ks[1], (DEPTH, D_MODEL), 0.02)
    w_in = nrm(ks[2], (DEPTH, D_MODEL, D_PROJ), D_MODEL ** -0.5)
    ssd_conv_w = nrm(ks[3], (DEPTH, SSD_CONV, SSD_CONV_DIM), SSD_CONV ** -0.5)
    ssd_conv_b = nrm(ks[4], (DEPTH, SSD_CONV_DIM), 0.01)
    dt0 = jnp.exp(jax.random.uniform(ks[5], (DEPTH, SSD_HEADS), f32, np.log(1e-3), np.log(1e-1)))
    ssd_dt_bias = dt0 + jnp.log(-jnp.expm1(-dt0))
    ssd_a_log = jnp.log(jax.random.uniform(ks[6], (DEPTH, SSD_HEADS), f32, 1.0, 16.0))
    ssd_d = 1.0 + nrm(ks[7], (DEPTH, SSD_HEADS), 0.02)
    ssd_norm_w = 1.0 + nrm(ks[8], (DEPTH, SSD_WIDTH), 0.02)
    lru_conv_w = nrm(ks[9], (DEPTH, LRU_CONV, LRU_WIDTH), LRU_CONV ** -0.5)
    lru_conv_b = nrm(ks[10], (DEPTH, LRU_WIDTH), 0.01)
    lru_w_a = nrm(ks[11], (DEPTH, LRU_BLOCKS, LRU_BLOCK_DIM, LRU_BLOCK_DIM), LRU_BLOCK_DIM ** -0.5)
    lru_b_a = nrm(ks[12], (DEPTH, LRU_WIDTH), 0.01)
    lru_w_x = nrm(ks[13], (DEPTH, LRU_BLOCKS, LRU_BLOCK_DIM, LRU_BLOCK_DIM), LRU_BLOCK_DIM ** -0.5)
    lru_b_x = nrm(ks[14], (DEPTH, LRU_WIDTH), 0.01)
    a0 = jax.random.uniform(ks[15], (DEPTH, LRU_WIDTH), f32, 0.9, 0.999) ** (1.0 / LRU_C)
    lru_lambda = jnp.log(a0) - jnp.log1p(-a0)
    w_out = nrm(ks[16], (DEPTH, D_MIX, D_MODEL), D_MIX ** -0.5)
    norm2_w = 1.0 + nrm(ks[17], (DEPTH, D_MODEL), 0.02)
    ffn_w_up = nrm(ks[18], (DEPTH, D_MODEL, 2 * D_FF), D_MODEL ** -0.5)
    ffn_conv_w = nrm(ks[19], (DEPTH, FFN_CONV, 2 * D_FF), FFN_CONV ** -0.5)
    ffn_conv_b = nrm(ks[20], (DEPTH, 2 * D_FF), 0.01)
    ffn_w_down = nrm(ks[21], (DEPTH, D_FF, D_MODEL), D_FF ** -0.5)
    final_norm_w = 1.0 + nrm(ks[22], (D_MODEL,), 0.02)
    return {'x': x, 'norm1_w': norm1_w, 'w_in': w_in,
            'ssd_conv_w': ssd_conv_w, 'ssd_conv_b': ssd_conv_b, 'ssd_dt_bias': ssd_dt_bias,
            'ssd_a_log': ssd_a_log, 'ssd_d': ssd_d, 'ssd_norm_w': ssd_norm_w,
            'lru_conv_w': lru_conv_w, 'lru_conv_b': lru_conv_b, 'lru_w_a': lru_w_a, 'lru_b_a': lru_b_a,
            'lru_w_x': lru_w_x, 'lru_b_x': lru_b_x, 'lru_lambda': lru_lambda,
            'w_out': w_out, 'norm2_w': norm2_w, 'ffn_w_up': ffn_w_up, 'ffn_conv_w': ffn_conv_w,
            'ffn_conv_b': ffn_conv_b, 'ffn_w_down': ffn_w_down, 'final_norm_w': final_norm_w}


def reference(x, norm1_w, w_in, ssd_conv_w, ssd_conv_b, ssd_dt_bias, ssd_a_log, ssd_d, ssd_norm_w,
              lru_conv_w, lru_conv_b, lru_w_a, lru_b_a, lru_w_x, lru_b_x, lru_lambda,
              w_out, norm2_w, ffn_w_up, ffn_conv_w, ffn_conv_b, ffn_w_down, final_norm_w):
    for l in range(DEPTH):
        h = rmsnorm(x, norm1_w[l])
        q, k, v, g, z, xbc, dt_raw, lru_gate, lru_x = split_proj(h @ w_in[l])
        y_ret = retention(q, k, v, g)
        y_ssd = ssd(z, xbc, dt_raw, ssd_conv_w[l], ssd_conv_b[l], ssd_dt_bias[l], ssd_a_log[l],
                    ssd_d[l], ssd_norm_w[l])
        y_lru = rglru(lru_gate, lru_x, lru_conv_w[l], lru_conv_b[l], lru_w_a[l], lru_b_a[l],
                      lru_w_x[l], lru_b_x[l], lru_lambda[l])
        y = jnp.concatenate([y_ret, y_ssd, y_lru], axis=-1)
        x = x + (y @ w_out[l]).astype(x.dtype)
        h = rmsnorm(x, norm2_w[l])
        x = x + conv_ffn(h, ffn_w_up[l], ffn_conv_w[l], ffn_conv_b[l], ffn_w_down[l]).astype(x.dtype)
    return rmsnorm(x, final_norm_w)
```

```python
import numpy as np
from contextlib import ExitStack
import concourse.bass as bass
import concourse.mybir as mybir
from concourse.bass_utils import run_bass_kernel_spmd

F32 = mybir.dt.float32
BF16 = mybir.dt.bfloat16
AF = mybir.ActivationFunctionType
ALU = mybir.AluOpType
AX = mybir.AxisListType

D = 1024
DPROJ = 3080
DFF = 2816
EPS = 1e-6
TT = 512
NST = 4
ENG = ['pe', 'act', 'dve', 'pool', 'sp']


class T:
    __slots__ = ('t', 'name', 'w', 'r', 'pool')

    def __init__(self, t, name, pool=None):
        self.t = t
        self.name = name
        self.w = None
        self.r = {}
        self.pool = pool

    def __getitem__(self, k):
        return self.t[k]


class FW:
    def __init__(self, nc, n_dma_sems=32):
        self.nc = nc
        self.es = ExitStack()
        self.eng = {'pe': nc.tensor, 'act': nc.scalar, 'dve': nc.vector, 'pool': nc.gpsimd, 'sp': nc.sync}
        self.sem = {e: self.es.enter_context(nc.semaphore('s_' + e)) for e in ENG}
        self.cnt = {e: 0 for e in ENG}
        self.waited = {e: {} for e in ENG}
        self.prog = {e: [] for e in ENG}
        self.dsem = [self.es.enter_context(nc.semaphore('d%d' % i)) for i in range(n_dma_sems)]
        self.dcnt = [0] * n_dma_sems
        self.dnext = 0
        self.n_hw = n_dma_sems
        self.ntile = 0

    def sb(self, shape, dtype, name=None):
        self.ntile += 1
        name = name or ('t%d' % self.ntile)
        t = self.es.enter_context(self.nc.sbuf_tensor(name, list(shape), dtype))
        return T(t, name)

    def ps(self, shape, dtype, name=None):
        self.ntile += 1
        name = name or ('p%d' % self.ntile)
        t = self.es.enter_context(self.nc.psum_tensor(name, list(shape), dtype))
        return T(t, name)

    def _need(self, e, dep):
        if dep is None:
            return
        if dep[0] == 'e':
            _, f, n = dep
            assert n <= self.cnt[f], "unissued ticket %s %d>%d (engine %s)" % (f, n, self.cnt[f], e)
            key = ('e', f)
            sem = self.sem[f]
        else:
            _, si, n = dep
            key = ('d', si)
            sem = self.dsem[si]
        if self.waited[e].get(key, 0) >= n:
            return
        self.waited[e][key] = n
        eng = self.eng[e]
        self.prog[e].append(lambda eng=eng, sem=sem, n=n: eng.wait_ge(sem, n))

    def op(self, e, fn, reads=(), writes=(), inc=True):
        pe = (e == 'pe')
        for t in reads:
            if t.w is not None and not (pe and t.w[0] == 'e' and t.w[1] == 'pe'):
                self._need(e, t.w)
        for t in writes:
            if t.w is not None and not (pe and t.w[0] == 'e' and t.w[1] == 'pe'):
                self._need(e, t.w)
            for k, d in t.r.items():
                if pe and d[0] == 'e' and d[1] == 'pe':
                    continue
                self._need(e, d)
        if inc:
            self.cnt[e] += 1
            n = self.cnt[e]
            sem = self.sem[e]
            self.prog[e].append(lambda eng=self.eng[e], fn=fn, sem=sem: fn(eng).then_inc(sem, 1))
        else:
            n = self.cnt[e] + 1
            self.prog[e].append(lambda eng=self.eng[e], fn=fn: fn(eng))
        dep = ('e', e, n)
        for t in reads:
            t.r[e] = dep
        for t in writes:
            t.w = dep
            t.r = {}
        return dep

    def dma(self, e, out_ap, in_ap, reads=(), writes=(), **kw):
        for t in reads:
            if t.w is not None:
                self._need(e, t.w)
        for t in writes:
            if t.w is not None:
                self._need(e, t.w)
            for k, d in t.r.items():
                self._need(e, d)
        if e == 'pool':
            self.dsem.append(self.es.enter_context(self.nc.semaphore('q%d' % len(self.dsem))))
            self.dcnt.append(0)
            semi = len(self.dsem) - 1
        else:
            semi = self.dnext
            self.dnext = (self.dnext + 1) % self.n_hw
            if self.dcnt[semi] > 0:
                self._need(e, ('d', semi, self.dcnt[semi]))
        self.dcnt[semi] += 16
        n = self.dcnt[semi]
        sem = self.dsem[semi]
        self.prog[e].append(lambda eng=self.eng[e], o=out_ap, i=in_ap, sem=sem, kw=kw:
                            eng.dma_start(out=o, in_=i, **kw).then_inc(sem, 16))
        dep = ('d', semi, n)
        for t in reads:
            t.r[('d', semi)] = dep
        for t in writes:
            t.w = dep
            t.r = {}
        return dep

    def wait_all_dma(self, e):
        for si in range(len(self.dsem)):
            if self.dcnt[si] > 0:
                self._need(e, ('d', si, self.dcnt[si]))

    def emit(self):
        nc = self.nc
        with nc.Block() as block:
            @block.tensor
            def _(t):
                for f in self.prog['pe']:
                    f()

            @block.scalar
            def _(t):
                for f in self.prog['act']:
                    f()

            @block.vector
            def _(t):
                for f in self.prog['dve']:
                    f()

            @block.gpsimd
            def _(t):
                for f in self.prog['pool']:
                    f()

            @block.sync
            def _(t):
                for f in self.prog['sp']:
                    f()
        self.es.close()


def _pack_layout():
    off = {}
    c = 0
    for name, n in [('n1w', 8), ('n2w', 8), ('scw', 32), ('scb', 8), ('dtb', 8), ('alog', 8), ('dsk', 8),
                    ('snw', 512), ('lcw', 8), ('lcb', 2), ('ba', 2), ('bx', 2), ('lam', 2),
                    ('fcw', 132), ('fcb', 44), ('wa', 256), ('wx', 256)]:
        off[name] = (c, n)
        c += n
    return off, c


POFF, PCOLS = _pack_layout()


def pack_params(inp, l):
    P = np.zeros((128, PCOLS), np.float32)

    def put(name, arr):
        o, n = POFF[name]
        assert arr.shape == (128, n), (name, arr.shape)
        P[:, o:o + n] = arr

    fm = lambda v: np.ascontiguousarray(v.reshape(-1, 128).T)
    bc = lambda v: np.ascontiguousarray(np.broadcast_to(v[None, :], (128, v.shape[0])))
    put('n1w', fm(inp['norm1_w'][l]))
    put('n2w', fm(inp['norm2_w'][l]))
    cw = inp['ssd_conv_w'][l]
    put('scw', np.ascontiguousarray(cw.reshape(4, 8, 128).transpose(2, 1, 0)).reshape(128, 32))
    put('scb', fm(inp['ssd_conv_b'][l]))
    put('dtb', bc(inp['ssd_dt_bias'][l]))
    put('alog', bc(inp['ssd_a_log'][l]))
    put('dsk', bc(inp['ssd_d'][l]))
    put('snw', bc(inp['ssd_norm_w'][l]))
    lw = inp['lru_conv_w'][l]
    put('lcw', np.ascontiguousarray(lw.reshape(4, 2, 128).transpose(2, 1, 0)).reshape(128, 8))
    put('lcb', fm(inp['lru_conv_b'][l]))
    put('ba', fm(inp['lru_b_a'][l]))
    put('bx', fm(inp['lru_b_x'][l]))
    put('lam', fm(inp['lru_lambda'][l]))
    fw_ = inp['ffn_conv_w'][l]
    put('fcw', np.ascontiguousarray(fw_.reshape(3, 44, 128).transpose(2, 1, 0)).reshape(128, 132))
    put('fcb', fm(inp['ffn_conv_b'][l]))
    for nm, key in (('wa', 'lru_w_a'), ('wx', 'lru_w_x')):
        w = inp[key][l]
        bd = np.zeros((128, 2, 128), np.float32)
        for k in range(4):
            c_, hh = k // 2, k % 2
            bd[hh * 64:(hh + 1) * 64, c_, hh * 64:(hh + 1) * 64] = w[k]
        put(nm, bd.reshape(128, 256))
    return P


def const_tables(L):
    nsub = L // 128
    half = 32
    inv = (10000.0 ** (-np.arange(half, dtype=np.float32) / np.float32(half))).astype(np.float32)
    pos = (np.arange(nsub)[None, :] * 128 + np.arange(128)[:, None]).astype(np.float32)
    ang = (pos[:, :, None] * inv[None, None, :]).astype(np.float32)
    cos = np.cos(ang).astype(np.float32).reshape(128, nsub * 32)
    sin = np.sin(ang).astype(np.float32).reshape(128, nsub * 32)
    lg = np.log(1.0 - 2.0 ** (-5.0 - np.arange(4, dtype=np.float64)))
    s = np.arange(128)[:, None]
    l_ = np.arange(128)[None, :]
    diff = l_ - s
    rdm = np.zeros((128, 4, 128), np.float64)
    for h in range(4):
        rdm[:, h, :] = np.where(diff >= 0, np.exp(lg[h] * np.maximum(diff, 0)), 0.0) * 0.125
    rqd = np.zeros((128, 2, 128), np.float64)
    rcd = np.zeros((128, 2), np.float64)
    for c in range(2):
        for hh in range(2):
            h = 2 * c + hh
            rqd[hh * 64:(hh + 1) * 64, c, :] = 0.125 * np.exp(lg[h] * (np.arange(128) + 1.0))[None, :]
            rcd[hh * 64:(hh + 1) * 64, c] = np.exp(lg[h] * 128.0)
    rkd = np.exp(lg[None, :] * (127.0 - np.arange(128))[:, None])
    return dict(cos=cos, sin=sin, rdm=rdm.reshape(128, 512).astype(np.float32),
                rqd=rqd.reshape(128, 256).astype(np.float32), rcd=rcd.astype(np.float32),
                rkd=rkd.astype(np.float32))


IN_SLABS = [(0, 512), (512, 512), (2560, 520), (1024, 512), (1536, 512), (2048, 512)]
UP_SLABS = []
for _s in range(6):
    _w = 512 if _s < 5 else 256
    UP_SLABS.append((_s * 512, _w))
    UP_SLABS.append((DFF + _s * 512, _w))
SLOTW = 4160


class StopBuild(Exception):
    pass


class Builder:
    stop = 99

    def ck(self, n):
        if self.stop == n:
            raise StopBuild()

    def __init__(self, nseq, L, layers, depth_total=2, final=True, dbg=None, nslots=4):
        self.nseq, self.L, self.layers, self.final = nseq, L, list(layers), final
        self.ntile = L // TT
        self.nsub = L // 128
        self.dbg = dbg or []
        nc = self.nc = bass.Bass("TRN2", target_bir_lowering=False)
        fw = self.fw = FW(nc)
        dt_in = lambda name, shape: nc.dram_tensor(name, list(shape), F32, kind="ExternalInput").ap()
        self.x = dt_in("x", [nseq, L, D])
        self.y = nc.dram_tensor("y", [nseq, L, D], F32, kind="ExternalOutput").ap()
        self.wd = {}
        for l in self.layers:
            self.wd[(l, 'in')] = dt_in("w_in%d" % l, [D, DPROJ])
            self.wd[(l, 'out')] = dt_in("w_out%d" % l, [D, D])
            self.wd[(l, 'up')] = dt_in("w_up%d" % l, [D, 2 * DFF])
            self.wd[(l, 'down')] = dt_in("w_down%d" % l, [DFF, D])
            self.wd[(l, 'par')] = dt_in("par%d" % l, [128, PCOLS])
        self.fnw_d = dt_in("fnw", [128, 8])
        self.cos_d = dt_in("cos", [128, self.nsub * 32])
        self.sin_d = dt_in("sin", [128, self.nsub * 32])
        self.rdm_d = dt_in("rdm", [128, 512])
        self.rqd_d = dt_in("rqd", [128, 256])
        self.rcd_d = dt_in("rcd", [128, 2])
        self.rkd_d = dt_in("rkd", [128, 4])
        self.dbg_out = {}
        for name, shape in self.dbg:
            self.dbg_out[name] = nc.dram_tensor("dbg_" + name, list(shape), F32, kind="ExternalOutput").ap()
        self.scr = {}
        for l in self.layers:
            for mat, slabs, kcn in (('in', IN_SLABS, 8), ('out', [(0, 512), (512, 512)], 8), ('up', UP_SLABS, 8),
                                    ('down', [(m * 128, 128) for m in range(8)], 22)):
                for s, (c0, cw) in enumerate(slabs):
                    t = nc.dram_tensor("scr_%d_%s_%d" % (l, mat, s), [128, kcn, cw], BF16).ap()
                    self.scr[(l, mat, s)] = (T(t, 'scr'), c0, cw, kcn)
        self.alloc_all(nslots)
        self.build()
        fw.emit()

    def alloc_all(self, nslots):
        fw = self.fw
        sb = fw.sb
        self.ident_f = sb([128, 128], F32)
        self.ident_b = sb([128, 128], BF16)
        self.U = sb([128, 128], F32)
        self.ones_f = sb([128, 128], F32)
        self.ones_b = sb([128, 128], BF16)
        self.cosT = sb([128, self.nsub * 32], F32)
        self.sinT = sb([128, self.nsub * 32], F32)
        self.rdm = sb([128, 512], F32)
        self.rqd = sb([128, 256], F32)
        self.rcd = sb([128, 2], F32)
        self.rkd = sb([128, 4], F32)
        self.fnw = sb([128, 8], F32)
        self.par = {l: sb([128, PCOLS], F32) for l in self.layers}
        self.aneg = {l: sb([128, 8], F32) for l in self.layers}
        self.DI = {l: sb([128, 8 * 128], BF16) for l in self.layers}
        self.cneg = {l: sb([128, 4], F32) for l in self.layers}
        self.wab = {l: sb([128, 256], BF16) for l in self.layers}
        self.wxb = {l: sb([128, 256], BF16) for l in self.layers}
        self.R = {l: sb([128, 128], F32) for l in self.layers}
        self.Rb = {l: sb([128, 128], BF16) for l in self.layers}
        self.S = {l: sb([128, 512], F32) for l in self.layers}
        self.Sb = {l: sb([128, 512], BF16) for l in self.layers}
        self.hst = {l: sb([128, 2], F32) for l in self.layers}
        self.shalo = {l: sb([128, 8 * 3], F32) for l in self.layers}
        self.lhalo = {l: sb([128, 2 * 3], F32) for l in self.layers}
        self.fhalo = {l: [sb([128, 88 * 2], F32) for _ in range(2)] for l in self.layers}
        self.xin = [sb([128, D], F32) for _ in range(2)]
        self.xin_i = 0
        self.ost = [sb([128, D], F32) for _ in range(2)]
        self.ost_i = 0
        self.xF = sb([128, 8 * TT], F32)
        self.xFc = [T(self.xF.t, 'xF%d' % m) for m in range(8)]
        self.h = sb([128, 8 * TT], BF16)
        self.yF = sb([128, 8 * TT], BF16)
        self.qkT = sb([128, 4 * TT], BF16)
        self.qz = sb([128, 4 * TT], BF16)
        self.qdz = sb([128, 4 * TT], BF16)
        self.wslot = [sb([128, SLOTW], BF16) for _ in range(nslots)]
        self.wslot_i = 0
        self.poolF = [T(sb([128, 515], F32).t, 'F%d' % i, 'F') for i in range(14)]
        self.poolH = [T(sb([128, 512], BF16).t, 'H%d' % i, 'H') for i in range(34)]
        self.poolS = [T(sb([128, 32], F32).t, 'S%d' % i, 'S') for i in range(20)]
        self.poolM = [T(sb([128, 128], BF16).t, 'M%d' % i, 'M') for i in range(16)]
        self.poolL = [T(sb([128, 128], F32).t, 'L%d' % i, 'L') for i in range(10)]
        self.pools = {'F': self.poolF, 'H': self.poolH, 'S': self.poolS, 'M': self.poolM, 'L': self.poolL}
        self.psb = [fw.ps([128, 512], F32) for _ in range(8)]
        self.ps_i = 0

    def get(self, p):
        lst = self.pools[p]
        assert lst, "pool %s empty" % p
        return lst.pop(0)

    def free(self, *ts):
        for t in ts:
            self.pools[t.pool].append(t)

    def nps(self):
        t = self.psb[self.ps_i]
        self.ps_i = (self.ps_i + 1) % 8
        return t

    def mm(self, out_ap, lhsT, rhs, start, stop, reads, writes, inc=None):
        if inc is None:
            inc = stop
        self.fw.op('pe', lambda t: t.matmul(out_ap, lhsT, rhs, start=start, stop=stop), reads, writes, inc=inc)

    def tr(self, out_ap, in_ap, ident_ap, reads, writes, inc=True):
        self.fw.op('pe', lambda t: t.transpose(out_ap, in_ap, ident_ap), reads, writes, inc=inc)

    def act(self, out_ap, in_ap, func, reads, writes, **kw):
        self.fw.op('act', lambda a: a.activation(out=out_ap, in_=in_ap, func=func, **kw), reads, writes)

    def tt(self, out_ap, in0, in1, op, reads, writes, eng='dve'):
        self.fw.op(eng, lambda v: v.tensor_tensor(out=out_ap, in0=in0, in1=in1, op=op), reads, writes)

    def ts(self, out_ap, in0, s1, s2, op0, op1, reads, writes, eng='dve'):
        if op1 is None:
            self.fw.op(eng, lambda v: v.tensor_scalar(out=out_ap, in0=in0, scalar1=s1, scalar2=None, op0=op0), reads, writes)
        else:
            self.fw.op(eng, lambda v: v.tensor_scalar(out=out_ap, in0=in0, scalar1=s1, scalar2=s2, op0=op0, op1=op1), reads, writes)

    def stt(self, out_ap, in0, scalar, in1, op0, op1, reads, writes):
        self.fw.op('dve', lambda v: v.scalar_tensor_tensor(out=out_ap, in0=in0, scalar=scalar, in1=in1, op0=op0, op1=op1), reads, writes)

    def cp(self, eng, out_ap, in_ap, reads, writes):
        if eng == 'act':
            self.fw.op('act', lambda a: a.copy(out=out_ap, in_=in_ap), reads, writes)
        else:
            self.fw.op(eng, lambda v: v.tensor_copy(out=out_ap, in_=in_ap), reads, writes)

    def memset(self, eng, t, ap, val):
        self.fw.op(eng, lambda v: v.memset(ap, val), [], [t])

    def P(self, l, name, a=None, b=None):
        o, n = POFF[name]
        if a is None:
            return self.par[l][:, o:o + n]
        return self.par[l][:, o + a:o + b]

    def dump(self, name, t, ap):
        if name in self.dbg_out:
            self.fw.dma('pool', self.dbg_out[name], ap, reads=[t])

    def build_wsched(self):
        sched = []
        for seq in range(self.nseq):
            for ti in range(self.ntile):
                for l in self.layers:
                    for s in range(6):
                        sched.append((l, 'in', s))
                    for s in range(2):
                        sched.append((l, 'out', s))
                    for s in range(12):
                        sched.append((l, 'up', s))
                    for s in range(8):
                        sched.append((l, 'down', s))
        self.wsched = sched
        self.w_loaded = 0
        self.w_used = 0
        self.w_slotof = {}

    def wload(self, key):
        assert self.wsched[self.w_used] == key, (self.wsched[self.w_used], key)
        ns = len(self.wslot)
        while self.w_loaded < len(self.wsched) and self.w_loaded < self.w_used + ns - 1:
            k = self.wsched[self.w_loaded]
            st, c0, cw, kcn = self.scr[k]
            slot = self.wslot[self.w_loaded % ns]
            self.fw.dma('sp', slot[:, 0:kcn * cw], st.t.rearrange("p k c -> p (k c)"), reads=[st], writes=[slot])
            self.w_loaded += 1
        slot = self.wslot[self.w_used % ns]
        self.w_used += 1
        return slot, self.scr[key][2]

    def build(self):
        fw = self.fw
        self.memset('pool', self.ones_f, self.ones_f[:], 1.0)
        self.memset('pool', self.qz, self.qz[:], 0.0)
        self.memset('pool', self.qdz, self.qdz[:], 0.0)
        self.memset('pool', self.ones_b, self.ones_b[:], 1.0)
        self.memset('pool', self.U, self.U[:], 1.0)
        fw.op('pool', lambda g: g.affine_select(out=self.U[:], in_=self.U[:], pattern=[[1, 128]], compare_op=ALU.is_ge,
                                                fill=0.0, base=0, channel_multiplier=-1), [self.U], [self.U])
        self.memset('pool', self.ident_f, self.ident_f[:], 1.0)
        fw.op('pool', lambda g: g.affine_select(out=self.ident_f[:], in_=self.ident_f[:], pattern=[[1, 128]], compare_op=ALU.is_equal,
                                                fill=0.0, base=0, channel_multiplier=-1), [self.ident_f], [self.ident_f])
        self.cp('dve', self.ident_b[:], self.ident_f[:], [self.ident_f], [self.ident_b])
        for t, d in ((self.cosT, self.cos_d), (self.sinT, self.sin_d), (self.rdm, self.rdm_d), (self.rqd, self.rqd_d),
                     (self.rcd, self.rcd_d), (self.rkd, self.rkd_d), (self.fnw, self.fnw_d)):
            fw.dma('sp', t[:], d, writes=[t])
        for l in self.layers:
            fw.dma('sp', self.par[l][:], self.wd[(l, 'par')], writes=[self.par[l]])
        for l in self.layers:
            for mat, n in (('in', 6), ('out', 2), ('up', 12), ('down', 8)):
                for s in range(n):
                    st, c0, cw, kcn = self.scr[(l, mat, s)]
                    src = self.wd[(l, mat)][:, c0:c0 + cw].rearrange("(k p) c -> p k c", p=128)
                    fw.dma('pool', st.t, src, writes=[st])
        for l in self.layers:
            par = self.par[l]
            self.act(self.aneg[l][:], self.P(l, 'alog'), AF.Exp, [par], [self.aneg[l]])
            self.ts(self.aneg[l][:], self.aneg[l][:], -1.0, None, ALU.mult, None, [self.aneg[l]], [self.aneg[l]])
            for hd in range(8):
                self.ts(self.DI[l][:, hd * 128:(hd + 1) * 128], self.ident_f[:], self.P(l, 'dsk', hd, hd + 1), None, ALU.mult, None,
                        [self.ident_f, par], [self.DI[l]])
            tmp = self.get('S')
            self.act(tmp[:, 0:2], self.P(l, 'lam'), AF.Exp, [par], [tmp], scale=-1.0)
            self.act(tmp[:, 0:2], tmp[:, 0:2], AF.Ln, [tmp], [tmp], bias=1.0)
            self.ts(self.cneg[l][:, 0:2], tmp[:, 0:2], -8.0, None, ALU.mult, None, [tmp], [self.cneg[l]])
            self.ts(self.cneg[l][:, 2:4], tmp[:, 0:2], -16.0, None, ALU.mult, None, [tmp], [self.cneg[l]])
            self.free(tmp)
            self.cp('dve', self.wab[l][:], self.P(l, 'wa'), [par], [self.wab[l]])
            self.cp('dve', self.wxb[l][:], self.P(l, 'wx'), [par], [self.wxb[l]])
        self.build_wsched()
        for seq in range(self.nseq):
            for l in self.layers:
                for t in (self.R[l], self.Rb[l], self.S[l], self.Sb[l], self.hst[l], self.shalo[l], self.lhalo[l], self.fhalo[l][0], self.fhalo[l][1]):
                    self.memset('pool', t, t[:], 0.0)
            for ti in range(self.ntile):
                self.load_x(seq, ti)
                try:
                    for l in self.layers:
                        self.layer(l, ti)
                except StopBuild:
                    pass
                self.store_out(seq, ti)
        fw.wait_all_dma('sp')

    def load_x(self, seq, ti):
        fw = self.fw
        for st in range(NST):
            xin = self.xin[self.xin_i]
            self.xin_i ^= 1
            t0 = ti * TT + st * 128
            fw.dma('sp', xin[:], self.x[seq, t0:t0 + 128, :], writes=[xin])
            for half in range(2):
                ps = self.nps()
                for k4 in range(4):
                    kc = half * 4 + k4
                    self.tr(ps[:, k4 * 128:(k4 + 1) * 128], xin[:, kc * 128:(kc + 1) * 128], self.ident_f[:], [xin, self.ident_f], [ps], inc=(k4 == 3))
                dst = self.xF[:, half * 4 * TT:(half + 1) * 4 * TT].rearrange("p (k t) -> p k t", k=4)[:, :, st * 128:(st + 1) * 128]
                self.cp('act', dst, ps[:, :].rearrange("p (k t) -> p k t", k=4), [ps], self.xFc[half * 4:(half + 1) * 4])

    def rms_rstd(self, src):
        sq = [self.get('H') for _ in range(8)]
        for kc in range(8):
            self.act(sq[kc][:, :], src[:, kc * TT:(kc + 1) * TT], AF.Square, [self.xFc[kc]], [sq[kc]])
        ps = self.nps()
        for kc in range(8):
            self.mm(ps[:, :], self.ones_b[:], sq[kc][:, :], kc == 0, kc == 7, [self.ones_b, sq[kc]], [ps])
        self.free(*sq)
        r = self.get('F')
        self.act(r[:, 0:TT], ps[:, :], AF.Ln, [ps], [r], scale=1.0 / D, bias=self.epsT[:, 0:1])
        self.act(r[:, 0:TT], r[:, 0:TT], AF.Exp, [r], [r], scale=-0.5)
        return r

    def norm_to_h(self, l, wname):
        r = self.rms_rstd(self.xF)
        for kc in range(8):
            self.stt(self.h[:, kc * TT:(kc + 1) * TT], self.xF[:, kc * TT:(kc + 1) * TT], self.P(l, wname, kc, kc + 1), r[:, 0:TT],
                     ALU.mult, ALU.mult, [self.xFc[kc], self.par[l], r], [self.h])
        self.free(r)

    def store_out(self, seq, ti):
        fw = self.fw
        if self.final:
            r = self.rms_rstd(self.xF)
            yo = [self.get('F') for _ in range(8)]
            for kc in range(8):
                self.stt(yo[kc][:, 0:TT], self.xF[:, kc * TT:(kc + 1) * TT], self.fnw[:, kc:kc + 1], r[:, 0:TT], ALU.mult, ALU.mult,
                         [self.xFc[kc], self.fnw, r], [yo[kc]])
            self.free(r)
            srcs = yo
            get = lambda kc, st: yo[kc][:, st * 128:(st + 1) * 128]
        else:
            srcs = list(self.xFc)
            get = lambda kc, st: self.xF[:, kc * TT + st * 128: kc * TT + (st + 1) * 128]
        for st in range(NST):
            ost = self.ost[self.ost_i]
            self.ost_i ^= 1
            for half in range(2):
                ps = self.nps()
                for k4 in range(4):
                    kc = half * 4 + k4
                    self.tr(ps[:, k4 * 128:(k4 + 1) * 128], get(kc, st), self.ident_f[:], [srcs[kc], self.ident_f], [ps], inc=(k4 == 3))
                self.cp('act', ost[:, half * 512:(half + 1) * 512], ps[:, :], [ps], [ost])
            t0 = ti * TT + st * 128
            fw.dma('act', self.y[seq, t0:t0 + 128, :], ost[:], reads=[ost])
        if self.final:
            self.free(*yo)

    def ffn_fin(self, accs, hid):
        au, av = accs
        self.act(au[:, 0:TT], au[:, 0:TT], AF.Gelu_apprx_tanh, [au], [au])
        o = self.get('H')
        self.tt(o[:, :], au[:, 0:TT], av[:, 0:TT], ALU.mult, [au, av], [o], eng='pool')
        self.free(au, av)
        hid.append(o)

    def layer(self, l, ti):
        par = self.par[l]
        h = self.h
        hk = lambda kc, a=0, b=TT: h[:, kc * TT + a: kc * TT + b]
        sub0 = ti * NST
        self.ck(0)
        self.norm_to_h(l, 'n1w')
        self.ck(1)
        W, cw = self.wload((l, 'in', 0))
        qk_TM = [self.get('H') for _ in range(NST)]
        for st in range(NST):
            ps = self.nps()
            for kc in range(8):
                self.mm(ps[:, :], hk(kc, st * 128, (st + 1) * 128), W[:, kc * cw: kc * cw + 512], kc == 0, kc == 7, [h, W], [ps])
            self.ck(101)
            v4 = ps[:, :].rearrange("p (h t d) -> p h t d", h=8, t=2)
            x1, x2 = v4[:, :, 0, :], v4[:, :, 1, :]
            o4 = qk_TM[st][:, :].rearrange("p (h t d) -> p h t d", h=8, t=2)
            sub = sub0 + st
            cosb = self.cosT[:, sub * 32:(sub + 1) * 32].unsqueeze(1).to_broadcast([128, 8, 32])
            sinb = self.sinT[:, sub * 32:(sub + 1) * 32].unsqueeze(1).to_broadcast([128, 8, 32])
            t1, t2 = self.get('F'), self.get('F')
            a1 = t1[:, 0:256].rearrange("p (h d) -> p h d", h=8)
            a2 = t2[:, 0:256].rearrange("p (h d) -> p h d", h=8)
            self.tt(a1, x1, cosb, ALU.mult, [ps, self.cosT], [t1])
            self.tt(a2, x2, sinb, ALU.mult, [ps, self.sinT], [t2])
            self.tt(o4[:, :, 0, :], a1, a2, ALU.subtract, [t1, t2], [qk_TM[st]])
            self.tt(a1, x1, sinb, ALU.mult, [ps, self.sinT], [t1])
            self.tt(a2, x2, cosb, ALU.mult, [ps, self.cosT], [t2])
            self.tt(o4[:, :, 1, :], a1, a2, ALU.add, [t1, t2], [qk_TM[st]])
            self.free(t1, t2)
            self.ck(102)
            pt = self.nps()
            ptb = pt[:, :].bitcast(BF16)
            for c in range(4):
                self.tr(ptb[:, c * 128:(c + 1) * 128], qk_TM[st][:, c * 128:(c + 1) * 128], self.ident_b[:], [qk_TM[st], self.ident_b], [pt], inc=(c == 3))
            self.ck(103)
            dst = self.qkT[:, 2 * TT:4 * TT].rearrange("p (c t) -> p c t", c=2)[:, :, st * 128:(st + 1) * 128]
            self.cp('act', dst, ptb[:, 256:512].rearrange("p (c t) -> p c t", c=2), [pt], [self.qkT])
            self.ck(104)
            for hh in range(2):
                pr = slice(hh * 64, (hh + 1) * 64)
                dz = self.qz[pr, :].rearrange("p (c h t) -> p c h t", c=2, h=2)[:, :, hh, st * 128:(st + 1) * 128]
                ddz = self.qdz[pr, :].rearrange("p (c h t) -> p c h t", c=2, h=2)[:, :, hh, st * 128:(st + 1) * 128]
                self.cp('act', dz, ptb[pr, 0:256].rearrange("p (c t) -> p c t", c=2), [pt], [self.qz])
                self.tt(ddz, dz, self.rqd[pr, :].rearrange("p (c t) -> p c t", c=2), ALU.mult, [self.qz, self.rqd], [self.qdz])
        self.ck(2)
        W, cw = self.wload((l, 'in', 1))
        v_TM = [self.get('H') for _ in range(NST)]
        sg = [self.get('F') for _ in range(NST)]
        for st in range(NST):
            ps = self.nps()
            for kc in range(8):
                self.mm(ps[:, :], hk(kc, st * 128, (st + 1) * 128), W[:, kc * cw: kc * cw + 512], kc == 0, kc == 7, [h, W], [ps])
            self.cp('act', v_TM[st][:, 0:256], ps[:, 0:256], [ps], [v_TM[st]])
            self.tt(v_TM[st][:, 256:512].rearrange("p (h d) -> p h d", h=4), ps[:, 0:256].rearrange("p (h d) -> p h d", h=4),
                    self.rkd[:, 0:4].unsqueeze(2).to_broadcast([128, 4, 64]), ALU.mult, [ps, self.rkd], [v_TM[st]])
            self.act(sg[st][:, 0:256], ps[:, 256:512], AF.Silu, [ps], [sg[st]])
        self.ck(3)
        R, Rb = self.R[l], self.Rb[l]
        for st in range(NST):
            sc = slice(st * 128, (st + 1) * 128)
            ps = self.nps()
            for hd in range(4):
                c, hh = hd // 2, hd % 2
                pr = slice(hh * 64, (hh + 1) * 64)
                self.mm(ps[:, hd * 128:(hd + 1) * 128], self.qkT[:, (2 + c) * TT + st * 128:(2 + c) * TT + (st + 1) * 128],
                        self.qz[:, hd * TT + st * 128: hd * TT + (st + 1) * 128], True, True, [self.qkT, self.qz], [ps], inc=(hd == 3))
            self.ck(201)
            PT = self.get('H')
            self.tt(PT[:, :], ps[:, :], self.rdm[:, :], ALU.mult, [ps, self.rdm], [PT])
            self.ck(202)
            py = self.nps()
            for hd in range(4):
                c, hh = hd // 2, hd % 2
                pr = slice(hh * 64, (hh + 1) * 64)
                self.mm(py[:, hd * 64:(hd + 1) * 64], PT[:, hd * 128:(hd + 1) * 128], v_TM[st][:, hd * 64:(hd + 1) * 64], True, False,
                        [PT, v_TM[st]], [py], inc=False)
                self.mm(py[:, hd * 64:(hd + 1) * 64], self.qdz[:, hd * TT + st * 128: hd * TT + (st + 1) * 128], Rb[:, c * 64:(c + 1) * 64],
                        False, True, [self.qdz, Rb], [py], inc=(hd == 3))
            self.free(PT)
            self.ck(203)
            pk = self.nps()
            for c in range(2):
                self.mm(pk[:, c * 128:(c + 1) * 128], qk_TM[st][:, 256 + c * 128: 256 + (c + 1) * 128], v_TM[st][:, 256 + c * 128: 256 + (c + 1) * 128],
                        True, True, [qk_TM[st], v_TM[st]], [pk], inc=(c == 1))
            self.ck(204)
            for c in range(2):
                for hh in range(2):
                    pr = slice(hh * 64, (hh + 1) * 64)
                    self.stt(R[pr, c * 64:(c + 1) * 64], R[pr, c * 64:(c + 1) * 64], self.rcd[pr, c:c + 1],
                             pk[pr, c * 128 + hh * 64: c * 128 + (hh + 1) * 64], ALU.mult, ALU.add, [R, self.rcd, pk], [R])
            self.ck(205)
            self.cp('act', Rb[:, :], R[:, :], [R], [Rb])
            self.ck(206)
            sm = self.get('S')
            y3 = py[:, 0:256].rearrange("p (h d) -> p h d", h=4)
            self.fw.op('dve', lambda v, sm=sm, y3=y3: v.tensor_reduce(out=sm[:, 0:4], in_=y3, axis=AX.X, op=ALU.add), [py], [sm])
            self.ck(207)
            ysq = self.get('F')
            self.act(ysq[:, 0:256], py[:, 0:256], AF.Square, [py], [ysq])
            self.fw.op('dve', lambda v, sm=sm, ysq=ysq: v.tensor_reduce(out=sm[:, 4:8], in_=ysq[:, 0:256].rearrange("p (h d) -> p h d", h=4),
                                                                      axis=AX.X, op=ALU.add), [ysq], [sm])
            self.ts(sm[:, 0:4], sm[:, 0:4], 1.0 / 64, None, ALU.mult, None, [sm], [sm])
            self.tt(sm[:, 8:12], sm[:, 0:4], sm[:, 0:4], ALU.mult, [sm], [sm])
            self.stt(sm[:, 4:8], sm[:, 4:8], 1.0 / 64, sm[:, 8:12], ALU.mult, ALU.subtract, [sm], [sm])
            self.act(sm[:, 4:8], sm[:, 4:8], AF.Ln, [sm], [sm], bias=self.epsT[:, 0:1])
            self.act(sm[:, 4:8], sm[:, 4:8], AF.Exp, [sm], [sm], scale=-0.5)
            self.ck(208)
            yc = ysq
            yc3 = yc[:, 0:256].rearrange("p (h d) -> p h d", h=4)
            self.tt(yc3, y3, sm[:, 0:4].unsqueeze(2).to_broadcast([128, 4, 64]), ALU.subtract, [py, sm], [yc])
            self.tt(yc3, yc3, sm[:, 4:8].unsqueeze(2).to_broadcast([128, 4, 64]), ALU.mult, [yc, sm], [yc])
            self.ck(209)
            yb = self.get('H')
            self.tt(yb[:, 0:256], yc[:, 0:256], sg[st][:, 0:256], ALU.mult, [yc, sg[st]], [yb])
            self.free(sm, ysq)
            pt = self.nps()
            ptb = pt[:, :].bitcast(BF16)
            for c in range(2):
                self.tr(ptb[:, c * 128:(c + 1) * 128], yb[:, c * 128:(c + 1) * 128], self.ident_b[:], [yb, self.ident_b], [pt], inc=(c == 1))
            self.free(yb)
            dst = self.yF[:, 0:2 * TT].rearrange("p (c t) -> p c t", c=2)[:, :, sc]
            self.cp('act', dst, ptb[:, 0:256].rearrange("p (c t) -> p c t", c=2), [pt], [self.yF])
        self.free(*qk_TM)
        self.free(*v_TM)
        self.free(*sg)
        self.ck(4)
        W, cw = self.wload((l, 'in', 2))
        pdt = self.nps()
        for st in range(NST):
            for kc in range(8):
                self.mm(pdt[:, st * 8:(st + 1) * 8], hk(kc, st * 128, (st + 1) * 128), W[:, kc * cw: kc * cw + 8], kc == 0, kc == 7,
                        [h, W], [pdt], inc=(kc == 7 and st == NST - 1))
        Sx, Sa, Sdt, SdA, Sln, Scum, Stot, Sncl, Secum, Sdec, Swg = [self.get('S') for _ in range(11)]
        b3 = lambda ap: ap.unsqueeze(1).to_broadcast([128, 4, 8])
        v3 = lambda t: t[:, 0:32].rearrange("p (s h) -> p s h", s=4)
        self.tt(v3(Sx), pdt[:, 0:32].rearrange("p (s h) -> p s h", s=4), b3(self.P(l, 'dtb')), ALU.add, [pdt, par], [Sx])
        self.act(Sa[:, 0:32], Sx[:, 0:32], AF.Abs, [Sx], [Sa])
        self.act(Sa[:, 0:32], Sa[:, 0:32], AF.Exp, [Sa], [Sa], scale=-1.0)
        self.act(Sa[:, 0:32], Sa[:, 0:32], AF.Ln, [Sa], [Sa], bias=1.0)
        self.stt(Sdt[:, 0:32], Sx[:, 0:32], 0.0, Sa[:, 0:32], ALU.max, ALU.add, [Sx, Sa], [Sdt])
        self.tt(v3(SdA), v3(Sdt), b3(self.aneg[l][:, 0:8]), ALU.mult, [Sdt, self.aneg[l]], [SdA])
        self.act(Sln[:, 0:32], Sdt[:, 0:32], AF.Ln, [Sdt], [Sln])
        pc = self.nps()
        self.mm(pc[:, 0:32], self.U[:], SdA[:, 0:32], True, True, [self.U, SdA], [pc], inc=False)
        self.mm(pc[:, 32:64], self.ones_f[:], SdA[:, 0:32], True, True, [self.ones_f, SdA], [pc])
        self.cp('act', Scum[:, 0:32], pc[:, 0:32], [pc], [Scum])
        self.cp('act', Stot[:, 0:32], pc[:, 32:64], [pc], [Stot])
        self.act(Secum[:, 0:32], Scum[:, 0:32], AF.Exp, [Scum], [Secum])
        self.act(Sdec[:, 0:32], Stot[:, 0:32], AF.Exp, [Stot], [Sdec])
        self.tt(Swg[:, 0:32], Stot[:, 0:32], Scum[:, 0:32], ALU.subtract, [Stot, Scum], [Swg])
        self.act(Swg[:, 0:32], Swg[:, 0:32], AF.Exp, [Swg], [Swg])
        self.tt(Swg[:, 0:32], Swg[:, 0:32], Sdt[:, 0:32], ALU.mult, [Swg, Sdt], [Swg])
        self.free(Sx, Sa, Stot, Sncl)
        self.ck(5)
        gg = []
        for c in range(2):
            ps = self.nps()
            for kc in range(8):
                self.mm(ps[:, :], W[:, kc * cw + 8 + c * 128: kc * cw + 8 + (c + 1) * 128], hk(kc), kc == 0, kc == 7, [W, h], [ps])
            g_ = self.get('F')
            self.act(g_[:, 0:TT], ps[:, :], AF.Gelu_apprx_tanh, [ps], [g_])
            gg.append(g_)
        for c in range(2):
            ps = self.nps()
            for kc in range(8):
                self.mm(ps[:, :], W[:, kc * cw + 264 + c * 128: kc * cw + 264 + (c + 1) * 128], hk(kc), kc == 0, kc == 7, [W, h], [ps])
            raw = self.get('F')
            halo = self.lhalo[l]
            self.cp('pool', raw[:, 0:3], halo[:, c * 3:(c + 1) * 3], [halo], [raw])
            self.cp('act', raw[:, 3:515], ps[:, :], [ps], [raw])
            xc = self.get('F')
            lcw = lambda k: self.P(l, 'lcw', c * 4 + k, c * 4 + k + 1)
            self.act(xc[:, 0:TT], raw[:, 3:515], AF.Identity, [raw, par], [xc], scale=lcw(3), bias=self.P(l, 'lcb', c, c + 1))
            for k in (2, 1, 0):
                self.stt(xc[:, 0:TT], raw[:, k:k + TT], lcw(k), xc[:, 0:TT], ALU.mult, ALU.add, [raw, par, xc], [xc])
            self.cp('pool', halo[:, c * 3:(c + 1) * 3], raw[:, 512:515], [raw], [halo])
            self.free(raw)
            xcb = self.get('H')
            self.cp('act', xcb[:, :], xc[:, 0:TT], [xc], [xcb])
            pr_ = self.nps()
            self.mm(pr_[:, :], self.wab[l][:, c * 128:(c + 1) * 128], xcb[:, :], True, True, [self.wab[l], xcb], [pr_])
            pi_ = self.nps()
            self.mm(pi_[:, :], self.wxb[l][:, c * 128:(c + 1) * 128], xcb[:, :], True, True, [self.wxb[l], xcb], [pi_])
            self.free(xcb)
            r_, i_, a_ = self.get('F'), self.get('F'), self.get('F')
            self.act(r_[:, 0:TT], pr_[:, :], AF.Sigmoid, [pr_, par], [r_], bias=self.P(l, 'ba', c, c + 1))
            self.act(i_[:, 0:TT], pi_[:, :], AF.Sigmoid, [pi_, par], [i_], bias=self.P(l, 'bx', c, c + 1))
            self.act(a_[:, 0:TT], r_[:, 0:TT], AF.Exp, [r_, self.cneg[l]], [a_], scale=self.cneg[l][:, c:c + 1])
            self.act(r_[:, 0:TT], r_[:, 0:TT], AF.Exp, [r_, self.cneg[l]], [r_], scale=self.cneg[l][:, 2 + c:3 + c])
            self.act(r_[:, 0:TT], r_[:, 0:TT], AF.Sqrt, [r_], [r_], scale=-1.0, bias=1.0)
            self.tt(i_[:, 0:TT], i_[:, 0:TT], xc[:, 0:TT], ALU.mult, [i_, xc], [i_])
            self.tt(i_[:, 0:TT], i_[:, 0:TT], r_[:, 0:TT], ALU.mult, [i_, r_], [i_])
            hs = self.hst[l]
            self.fw.op('dve', lambda v, xc=xc, a_=a_, i_=i_, hs=hs, c=c: v.tensor_tensor_scan(
                out=xc[:, 0:TT], data0=a_[:, 0:TT], data1=i_[:, 0:TT], initial=hs[:, c:c + 1], op0=ALU.mult, op1=ALU.add),
                [a_, i_, hs], [xc])
            self.cp('dve', hs[:, c:c + 1], xc[:, TT - 1:TT], [xc], [hs])
            self.tt(self.yF[:, (6 + c) * TT:(7 + c) * TT], xc[:, 0:TT], gg[c][:, 0:TT], ALU.mult, [xc, gg[c]], [self.yF])
            self.free(r_, i_, a_, xc, gg[c])
        self.ck(6)
        W, cw = self.wload((l, 'in', 3))
        sz = [self.get('F') for _ in range(NST)]
        for st in range(NST):
            ps = self.nps()
            for kc in range(8):
                self.mm(ps[:, :], hk(kc, st * 128, (st + 1) * 128), W[:, kc * cw: kc * cw + 512], kc == 0, kc == 7, [h, W], [ps])
            self.act(sz[st][:, 0:TT], ps[:, :], AF.Silu, [ps], [sz[st]])
        self.ck(7)
        xbcF = []
        for slab in (4, 5):
            W, cw = self.wload((l, 'in', slab))
            for m in range(4):
                cidx = (slab - 4) * 4 + m
                ps = self.nps()
                for kc in range(8):
                    self.mm(ps[:, :], W[:, kc * cw + m * 128: kc * cw + (m + 1) * 128], hk(kc), kc == 0, kc == 7, [W, h], [ps])
                raw = self.get('F')
                halo = self.shalo[l]
                self.cp('pool', raw[:, 0:3], halo[:, cidx * 3:(cidx + 1) * 3], [halo], [raw])
                self.cp('act', raw[:, 3:515], ps[:, :], [ps], [raw])
                acc = self.get('F')
                scw = lambda k: self.P(l, 'scw', cidx * 4 + k, cidx * 4 + k + 1)
                self.act(acc[:, 0:TT], raw[:, 3:515], AF.Identity, [raw, par], [acc], scale=scw(3), bias=self.P(l, 'scb', cidx, cidx + 1))
                for k in (2, 1, 0):
                    self.stt(acc[:, 0:TT], raw[:, k:k + TT], scw(k), acc[:, 0:TT], ALU.mult, ALU.add, [raw, par, acc], [acc])
                self.cp('pool', halo[:, cidx * 3:(cidx + 1) * 3], raw[:, 512:515], [raw], [halo])
                self.free(raw)
                o = self.get('H')
                self.act(o[:, :], acc[:, 0:TT], AF.Silu, [acc], [o])
                self.free(acc)
                xbcF.append(o)
        xsF, BF, CF = xbcF[0:4], xbcF[4:6], xbcF[6:8]
        self.ck(8)
        S, Sb = self.S[l], self.Sb[l]

        def ssd_A(st):
            sc = slice(st * 128, (st + 1) * 128)
            xs_TM = self.get('H')
            pt = self.nps()
            ptb = pt[:, :].bitcast(BF16)
            for c in range(4):
                self.tr(ptb[:, c * 128:(c + 1) * 128], xsF[c][:, sc], self.ident_b[:], [xsF[c], self.ident_b], [pt], inc=(c == 3))
            self.cp('act', xs_TM[:, :], ptb[:, 0:512], [pt], [xs_TM])
            B_TM = self.get('H')
            pt = self.nps()
            ptb = pt[:, :].bitcast(BF16)
            for g in range(2):
                self.tr(ptb[:, g * 128:(g + 1) * 128], BF[g][:, sc], self.ident_b[:], [BF[g], self.ident_b], [pt], inc=(g == 1))
            self.cp('act', B_TM[:, 0:256], ptb[:, 0:256], [pt], [B_TM])
            pcb = self.nps()
            for g in range(2):
                self.mm(pcb[:, g * 128:(g + 1) * 128], BF[g][:, sc], CF[g][:, sc], True, True, [BF[g], CF[g]], [pcb], inc=(g == 1))
            CBm = [self.get('L') for _ in range(2)]
            for g in range(2):
                self.tt(CBm[g][:, :], pcb[:, g * 128:(g + 1) * 128], self.U[:, :], ALU.mult, [pcb, self.U], [CBm[g]])
            MT = []
            Lrs = []
            pqs = []
            for hq in range(2):
                pq = self.nps()
                for j in range(4):
                    hd = hq * 4 + j
                    col = st * 8 + hd
                    self.mm(pq[:, j * 128:(j + 1) * 128], SdA[:, col:col + 1].to_broadcast([128, 128]), self.U[:], True, True,
                            [SdA, self.U], [pq], inc=(j == 3))
                pqs.append(pq)
            for hd in range(8):
                col = st * 8 + hd
                pq, j = pqs[hd // 4], hd % 4
                Lr = self.get('L')
                self.ts(Lr[:, :], pq[:, j * 128:(j + 1) * 128], Scum[:, col:col + 1], 0.0, ALU.subtract, ALU.min, [pq, Scum], [Lr])
                Lrs.append(Lr)
            for hd in range(8):
                col = st * 8 + hd
                self.act(Lrs[hd][:, :], Lrs[hd][:, :], AF.Exp, [Lrs[hd], Sln], [Lrs[hd]], bias=Sln[:, col:col + 1])
            for hd in range(8):
                m_ = self.get('M')
                self.tt(m_[:, :], Lrs[hd][:, :], CBm[hd // 4][:, :], ALU.mult, [Lrs[hd], CBm[hd // 4]], [m_])
                MT.append(m_)
            self.free(*Lrs)
            self.free(*CBm)
            xw = self.get('H')
            self.tt(xw[:, :].rearrange("p (h d) -> p h d", h=8), xs_TM[:, :].rearrange("p (h d) -> p h d", h=8),
                    Swg[:, st * 8:(st + 1) * 8].unsqueeze(2).to_broadcast([128, 8, 64]), ALU.mult, [xs_TM, Swg], [xw])
            return xs_TM, B_TM, MT, xw

        def ssd_B(st, xs_TM, B_TM, MT, xw):
            sc = slice(st * 128, (st + 1) * 128)
            pY = self.nps()
            for hd in range(8):
                hs_ = slice(hd * 64, (hd + 1) * 64)
                self.mm(pY[:, hs_], MT[hd][:, :], xs_TM[:, hs_], True, False, [MT[hd], xs_TM], [pY], inc=False)
                self.mm(pY[:, hs_], self.DI[l][:, hd * 128:(hd + 1) * 128], xs_TM[:, hs_], False, True, [self.DI[l], xs_TM], [pY], inc=(hd == 7))
            self.free(*MT)
            pO = self.nps()
            for g in range(2):
                self.mm(pO[:, g * 256:(g + 1) * 256], CF[g][:, sc], Sb[:, g * 256:(g + 1) * 256], True, True, [CF[g], Sb], [pO], inc=(g == 1))
            pS = self.nps()
            for g in range(2):
                self.mm(pS[:, g * 256:(g + 1) * 256], B_TM[:, g * 128:(g + 1) * 128], xw[:, g * 256:(g + 1) * 256], True, True, [B_TM, xw], [pS], inc=(g == 1))
            self.free(xw, B_TM, xs_TM)
            S3 = S[:, :].rearrange("p (h d) -> p h d", h=8)
            self.tt(S3, S3, Sdec[:, st * 8:(st + 1) * 8].unsqueeze(2).to_broadcast([128, 8, 64]), ALU.mult, [S, Sdec], [S])
            self.tt(S[:, :], S[:, :], pS[:, :], ALU.add, [S, pS], [S])
            self.cp('act', Sb[:, :], S[:, :], [S], [Sb])
            Y = self.get('F')
            self.tt(Y[:, 0:TT].rearrange("p (h d) -> p h d", h=8), pO[:, :].rearrange("p (h d) -> p h d", h=8),
                    Secum[:, st * 8:(st + 1) * 8].unsqueeze(2).to_broadcast([128, 8, 64]), ALU.mult, [pO, Secum], [Y])
            self.tt(Y[:, 0:TT], Y[:, 0:TT], pY[:, :], ALU.add, [Y, pY], [Y])
            self.tt(Y[:, 0:TT], Y[:, 0:TT], sz[st][:, 0:TT], ALU.mult, [Y, sz[st]], [Y])
            junk = self.get('F')
            ss = self.get('S')
            for g in range(2):
                self.fw.op('act', lambda a, junk=junk, Y=Y, ss=ss, g=g: a.activation(out=junk[:, g * 256:(g + 1) * 256], in_=Y[:, g * 256:(g + 1) * 256],
                                                                                  func=AF.Square, accum_out=ss[:, g:g + 1]), [Y], [junk, ss])
            self.free(junk)
            self.act(ss[:, 0:2], ss[:, 0:2], AF.Ln, [ss], [ss], scale=1.0 / 256, bias=self.epsT[:, 0:1])
            self.act(ss[:, 0:2], ss[:, 0:2], AF.Exp, [ss], [ss], scale=-0.5)
            yb = self.get('H')
            o_, n_ = POFF['snw']
            for g in range(2):
                self.stt(yb[:, g * 256:(g + 1) * 256], Y[:, g * 256:(g + 1) * 256], ss[:, g:g + 1], self.par[l][:, o_ + g * 256: o_ + (g + 1) * 256],
                         ALU.mult, ALU.mult, [Y, ss, par], [yb])
            self.free(Y, ss)
            pt = self.nps()
            ptb = pt[:, :].bitcast(BF16)
            for c in range(4):
                self.tr(ptb[:, c * 128:(c + 1) * 128], yb[:, c * 128:(c + 1) * 128], self.ident_b[:], [yb, self.ident_b], [pt], inc=(c == 3))
            self.free(yb)
            dst = self.yF[:, 2 * TT:6 * TT].rearrange("p (c t) -> p c t", c=4)[:, :, sc]
            self.cp('act', dst, ptb[:, 0:512].rearrange("p (c t) -> p c t", c=4), [pt], [self.yF])

        stA = ssd_A(0)
        for st in range(NST):
            nxt = ssd_A(st + 1) if st + 1 < NST else None
            ssd_B(st, *stA)
            stA = nxt
        self.free(*xbcF)
        self.free(*sz)
        self.free(Sdt, SdA, Sln, Scum, Secum, Sdec, Swg)
        self.dump('yF', self.yF, self.yF[:, :])
        self.ck(9)
        for m in range(8):
            if m % 4 == 0:
                W, cw = self.wload((l, 'out', m // 4))
            ps = self.nps()
            for kc in range(8):
                self.mm(ps[:, :], W[:, kc * cw + (m % 4) * 128: kc * cw + (m % 4 + 1) * 128], self.yF[:, kc * TT:(kc + 1) * TT], kc == 0, kc == 7,
                        [W, self.yF], [ps])
            self.tt(self.xF[:, m * TT:(m + 1) * TT], ps[:, :], self.xF[:, m * TT:(m + 1) * TT], ALU.add, [ps, self.xFc[m]], [self.xFc[m]])
        self.dump('x1', self.xF, self.xF[:, :])
        self.ck(10)
        self.norm_to_h(l, 'n2w')
        hid = []
        pend = []
        fh = self.fhalo[l][ti % 2]
        fhn = self.fhalo[l][(ti + 1) % 2]
        for s in range(6):
            Wu, cwu = self.wload((l, 'up', 2 * s))
            Wv, cwv = self.wload((l, 'up', 2 * s + 1))
            for jj in range(cwu // 128):
                j = 4 * s + jj
                accs = []
                for (Wx, cwx, ci) in ((Wu, cwu, j), (Wv, cwv, 22 + j)):
                    ps = self.nps()
                    for kc in range(8):
                        self.mm(ps[:, :], Wx[:, kc * cwx + jj * 128: kc * cwx + (jj + 1) * 128], hk(kc), kc == 0, kc == 7, [Wx, h], [ps])
                    acc = self.get('F')
                    fcw = lambda k, ci=ci: self.P(l, 'fcw', ci * 3 + k, ci * 3 + k + 1)
                    self.act(acc[:, 0:TT], ps[:, :], AF.Identity, [ps, par], [acc], scale=fcw(2), bias=self.P(l, 'fcb', ci, ci + 1))
                    self.cp('act', fhn[:, ci * 2: ci * 2 + 2], ps[:, TT - 2:TT], [ps], [fhn])
                    self.stt(acc[:, 1:TT], ps[:, 0:TT - 1], fcw(1), acc[:, 1:TT], ALU.mult, ALU.add, [ps, par, acc], [acc])
                    self.stt(acc[:, 2:TT], ps[:, 0:TT - 2], fcw(0), acc[:, 2:TT], ALU.mult, ALU.add, [ps, par, acc], [acc])
                    hl = fh[:, ci * 2: ci * 2 + 2]
                    self.stt(acc[:, 0:1], fh[:, ci * 2 + 1: ci * 2 + 2], fcw(1), acc[:, 0:1], ALU.mult, ALU.add, [fh, par, acc], [acc])
                    self.stt(acc[:, 0:2], hl, fcw(0), acc[:, 0:2], ALU.mult, ALU.add, [fh, par, acc], [acc])
                    accs.append(acc)
                pend.append(tuple(accs))
                if len(pend) > 1:
                    self.ffn_fin(pend.pop(0), hid)
        while pend:
            self.ffn_fin(pend.pop(0), hid)
        self.ck(11)
        for m in range(8):
            W, cw = self.wload((l, 'down', m))
            ps = self.nps()
            for kc in range(22):
                self.mm(ps[:, :], W[:, kc * cw: (kc + 1) * cw], hid[kc][:, :], kc == 0, kc == 21, [W, hid[kc]], [ps])
            self.tt(self.xF[:, m * TT:(m + 1) * TT], ps[:, :], self.xF[:, m * TT:(m + 1) * TT], ALU.add, [ps, self.xFc[m]], [self.xFc[m]])
        self.free(*hid)
        self.dump('x2', self.xF, self.xF[:, :])


def _mk_eps(b):
    b.epsT = b.fw.sb([128, 1], F32)
    b.memset('pool', b.epsT, b.epsT[:], EPS)


_orig_alloc = Builder.alloc_all


def _alloc_all(self, nslots):
    _orig_alloc(self, nslots)
    _mk_eps(self)


Builder.alloc_all = _alloc_all


def make_in_maps(inputs, nseq, L, layers, ncores):
    consts = const_tables(L)
    maps = []
    shared = {}
    for l in layers:
        shared["w_in%d" % l] = np.ascontiguousarray(inputs['w_in'][l])
        shared["w_out%d" % l] = np.ascontiguousarray(inputs['w_out'][l])
        shared["w_up%d" % l] = np.ascontiguousarray(inputs['ffn_w_up'][l])
        shared["w_down%d" % l] = np.ascontiguousarray(inputs['ffn_w_down'][l])
        shared["par%d" % l] = pack_params(inputs, l)
    shared["fnw"] = np.ascontiguousarray(inputs['final_norm_w'].reshape(8, 128).T)
    for k, v in consts.items():
        shared[k] = v
    return shared


N_CORES = 8


def kernel(**inputs):
    inputs = {k: np.asarray(v) for k, v in inputs.items()}
    x = inputs['x']
    Bt, L, _ = x.shape
    nseq = Bt // N_CORES
    layers = list(range(inputs['w_in'].shape[0]))
    b = Builder(nseq, L, layers, final=True)
    shared = make_in_maps(inputs, nseq, L, layers, N_CORES)
    in_maps = []
    for c in range(N_CORES):
        m = dict(shared)
        m["x"] = np.ascontiguousarray(x[c * nseq:(c + 1) * nseq])
        in_maps.append(m)
    res = run_bass_kernel_spmd(b.nc, in_maps, core_ids=list(range(N_CORES)))
    out = np.concatenate([np.asarray(r["y"]) for r in res.results], axis=0)
    return out.astype(np.float32)
```

```python
import numpy as np
from contextlib import ExitStack
import concourse.bass as bass
import concourse.mybir as mybir
from concourse.bass_utils import run_bass_kernel_spmd

F32 = mybir.dt.float32
BF16 = mybir.dt.bfloat16
AF = mybir.ActivationFunctionType
ALU = mybir.AluOpType
AX = mybir.AxisListType

D = 1024
DPROJ = 3080
DFF = 2816
EPS = 1e-6
TT = 512
NST = 4
ENG = ['pe', 'act', 'dve', 'pool', 'sp']


class T:
    __slots__ = ('t', 'name', 'w', 'r', 'pool')

    def __init__(self, t, name, pool=None):
        self.t = t
        self.name = name
        self.w = None
        self.r = {}
        self.pool = pool

    def __getitem__(self, k):
        return self.t[k]


class FW:
    def __init__(self, nc, n_dma_sems=32):
        self.nc = nc
        self.es = ExitStack()
        self.eng = {'pe': nc.tensor, 'act': nc.scalar, 'dve': nc.vector, 'pool': nc.gpsimd, 'sp': nc.sync}
        self.sem = {e: self.es.enter_context(nc.semaphore('s_' + e)) for e in ENG}
        self.cnt = {e: 0 for e in ENG}
        self.waited = {e: {} for e in ENG}
        self.prog = {e: [] for e in ENG}
        self.dsem = [self.es.enter_context(nc.semaphore('d%d' % i)) for i in range(n_dma_sems)]
        self.dcnt = [0] * n_dma_sems
        self.dnext = 0
        self.n_hw = n_dma_sems
        self.ntile = 0

    def sb(self, shape, dtype, name=None):
        self.ntile += 1
        name = name or ('t%d' % self.ntile)
        t = self.es.enter_context(self.nc.sbuf_tensor(name, list(shape), dtype))
        return T(t, name)

    def ps(self, shape, dtype, name=None):
        self.ntile += 1
        name = name or ('p%d' % self.ntile)
        t = self.es.enter_context(self.nc.psum_tensor(name, list(shape), dtype))
        return T(t, name)

    def _need(self, e, dep):
        if dep is None:
            return
        if dep[0] == 'e':
            _, f, n = dep
            assert n <= self.cnt[f], "unissued ticket %s %d>%d (engine %s)" % (f, n, self.cnt[f], e)
            key = ('e', f)
            sem = self.sem[f]
        else:
            _, si, n = dep
            key = ('d', si)
            sem = self.dsem[si]
        if self.waited[e].get(key, 0) >= n:
            return
        self.waited[e][key] = n
        eng = self.eng[e]
        self.prog[e].append(lambda eng=eng, sem=sem, n=n: eng.wait_ge(sem, n))

    def op(self, e, fn, reads=(), writes=(), inc=True):
        pe = (e == 'pe')
        for t in reads:
            if t.w is not None and not (pe and t.w[0] == 'e' and t.w[1] == 'pe'):
                self._need(e, t.w)
        for t in writes:
            if t.w is not None and not (pe and t.w[0] == 'e' and t.w[1] == 'pe'):
                self._need(e, t.w)
            for k, d in t.r.items():
                if pe and d[0] == 'e' and d[1] == 'pe':
                    continue
                self._need(e, d)
        if inc:
            self.cnt[e] += 1
            n = self.cnt[e]
            sem = self.sem[e]
            self.prog[e].append(lambda eng=self.eng[e], fn=fn, sem=sem: fn(eng).then_inc(sem, 1))
        else:
            n = self.cnt[e] + 1
            self.prog[e].append(lambda eng=self.eng[e], fn=fn: fn(eng))
        dep = ('e', e, n)
        for t in reads:
            t.r[e] = dep
        for t in writes:
            t.w = dep
            t.r = {}
        return dep

    def dma(self, e, out_ap, in_ap, reads=(), writes=(), **kw):
        for t in reads:
            if t.w is not None:
                self._need(e, t.w)
        for t in writes:
            if t.w is not None:
                self._need(e, t.w)
            for k, d in t.r.items():
                self._need(e, d)
        if e == 'pool':
            self.dsem.append(self.es.enter_context(self.nc.semaphore('q%d' % len(self.dsem))))
            self.dcnt.append(0)
            semi = len(self.dsem) - 1
        else:
            semi = self.dnext
            self.dnext = (self.dnext + 1) % self.n_hw
            if self.dcnt[semi] > 0:
                self._need(e, ('d', semi, self.dcnt[semi]))
        self.dcnt[semi] += 16
        n = self.dcnt[semi]
        sem = self.dsem[semi]
        self.prog[e].append(lambda eng=self.eng[e], o=out_ap, i=in_ap, sem=sem, kw=kw:
                            eng.dma_start(out=o, in_=i, **kw).then_inc(sem, 16))
        dep = ('d', semi, n)
        for t in reads:
            t.r[('d', semi)] = dep
        for t in writes:
            t.w = dep
            t.r = {}
        return dep

    def wait_all_dma(self, e):
        for si in range(len(self.dsem)):
            if self.dcnt[si] > 0:
                self._need(e, ('d', si, self.dcnt[si]))

    def emit(self):
        nc = self.nc
        with nc.Block() as block:
            @block.tensor
            def _(t):
                for f in self.prog['pe']:
                    f()

            @block.scalar
            def _(t):
                for f in self.prog['act']:
                    f()

            @block.vector
            def _(t):
                for f in self.prog['dve']:
                    f()

            @block.gpsimd
            def _(t):
                for f in self.prog['pool']:
                    f()

            @block.sync
            def _(t):
                for f in self.prog['sp']:
                    f()
        self.es.close()


def _pack_layout():
    off = {}
    c = 0
    for name, n in [('n1w', 8), ('n2w', 8), ('scw', 32), ('scb', 8), ('dtb', 8), ('alog', 8), ('dsk', 8),
                    ('snw', 512), ('lcw', 8), ('lcb', 2), ('ba', 2), ('bx', 2), ('lam', 2),
                    ('fcw', 132), ('fcb', 44), ('wa', 256), ('wx', 256)]:
        off[name] = (c, n)
        c += n
    return off, c


POFF, PCOLS = _pack_layout()


def pack_params(inp, l):
    P = np.zeros((128, PCOLS), np.float32)

    def put(name, arr):
        o, n = POFF[name]
        assert arr.shape == (128, n), (name, arr.shape)
        P[:, o:o + n] = arr

    fm = lambda v: np.ascontiguousarray(v.reshape(-1, 128).T)
    bc = lambda v: np.ascontiguousarray(np.broadcast_to(v[None, :], (128, v.shape[0])))
    put('n1w', fm(inp['norm1_w'][l]))
    put('n2w', fm(inp['norm2_w'][l]))
    cw = inp['ssd_conv_w'][l]
    put('scw', np.ascontiguousarray(cw.reshape(4, 8, 128).transpose(2, 1, 0)).reshape(128, 32))
    put('scb', fm(inp['ssd_conv_b'][l]))
    put('dtb', bc(inp['ssd_dt_bias'][l]))
    put('alog', bc(inp['ssd_a_log'][l]))
    put('dsk', bc(inp['ssd_d'][l]))
    put('snw', bc(inp['ssd_norm_w'][l]))
    lw = inp['lru_conv_w'][l]
    put('lcw', np.ascontiguousarray(lw.reshape(4, 2, 128).transpose(2, 1, 0)).reshape(128, 8))
    put('lcb', fm(inp['lru_conv_b'][l]))
    put('ba', fm(inp['lru_b_a'][l]))
    put('bx', fm(inp['lru_b_x'][l]))
    put('lam', fm(inp['lru_lambda'][l]))
    fw_ = inp['ffn_conv_w'][l]
    put('fcw', np.ascontiguousarray(fw_.reshape(3, 44, 128).transpose(2, 1, 0)).reshape(128, 132))
    put('fcb', fm(inp['ffn_conv_b'][l]))
    for nm, key in (('wa', 'lru_w_a'), ('wx', 'lru_w_x')):
        w = inp[key][l]
        bd = np.zeros((128, 2, 128), np.float32)
        for k in range(4):
            c_, hh = k // 2, k % 2
            bd[hh * 64:(hh + 1) * 64, c_, hh * 64:(hh + 1) * 64] = w[k]
        put(nm, bd.reshape(128, 256))
    return P


def const_tables(L):
    nsub = L // 128
    half = 32
    inv = (10000.0 ** (-np.arange(half, dtype=np.float32) / np.float32(half))).astype(np.float32)
    pos = (np.arange(nsub)[None, :] * 128 + np.arange(128)[:, None]).astype(np.float32)
    ang = (pos[:, :, None] * inv[None, None, :]).astype(np.float32)
    cos = np.cos(ang).astype(np.float32).reshape(128, nsub * 32)
    sin = np.sin(ang).astype(np.float32).reshape(128, nsub * 32)
    lg = np.log(1.0 - 2.0 ** (-5.0 - np.arange(4, dtype=np.float64)))
    s = np.arange(128)[:, None]
    l_ = np.arange(128)[None, :]
    diff = l_ - s
    rdm = np.zeros((128, 4, 128), np.float64)
    for h in range(4):
        rdm[:, h, :] = np.where(diff >= 0, np.exp(lg[h] * np.maximum(diff, 0)), 0.0) * 0.125
    rqd = np.zeros((128, 2, 128), np.float64)
    rcd = np.zeros((128, 2), np.float64)
    for c in range(2):
        for hh in range(2):
            h = 2 * c + hh
            rqd[hh * 64:(hh + 1) * 64, c, :] = 0.125 * np.exp(lg[h] * (np.arange(128) + 1.0))[None, :]
            rcd[hh * 64:(hh + 1) * 64, c] = np.exp(lg[h] * 128.0)
    rkd = np.exp(lg[None, :] * (127.0 - np.arange(128))[:, None])
    return dict(cos=cos, sin=sin, rdm=rdm.reshape(128, 512).astype(np.float32),
                rqd=rqd.reshape(128, 256).astype(np.float32), rcd=rcd.astype(np.float32),
                rkd=rkd.astype(np.float32))


IN_SLABS = [(0, 512), (512, 512), (2560, 520), (1024, 512), (1536, 512), (2048, 512)]
UP_SLABS = []
for _s in range(6):
    _w = 512 if _s < 5 else 256
    UP_SLABS.append((_s * 512, _w))
    UP_SLABS.append((DFF + _s * 512, _w))
SLOTW = 4160


class StopBuild(Exception):
    pass


class Builder:
    stop = 99

    def ck(self, n):
        if self.stop == n:
            raise StopBuild()

    def __init__(self, nseq, L, layers, depth_total=2, final=True, dbg=None, nslots=4):
        self.nseq, self.L, self.layers, self.final = nseq, L, list(layers), final
        self.ntile = L // TT
        self.nsub = L // 128
        self.dbg = dbg or []
        nc = self.nc = bass.Bass("TRN2", target_bir_lowering=False)
        fw = self.fw = FW(nc)
        dt_in = lambda name, shape: nc.dram_tensor(name, list(shape), F32, kind="ExternalInput").ap()
        self.x = dt_in("x", [nseq, L, D])
        self.y = nc.dram_tensor("y", [nseq, L, D], F32, kind="ExternalOutput").ap()
        self.wd = {}
        for l in self.layers:
            self.wd[(l, 'in')] = dt_in("w_in%d" % l, [D, DPROJ])
            self.wd[(l, 'out')] = dt_in("w_out%d" % l, [D, D])
            self.wd[(l, 'up')] = dt_in("w_up%d" % l, [D, 2 * DFF])
            self.wd[(l, 'down')] = dt_in("w_down%d" % l, [DFF, D])
            self.wd[(l, 'par')] = dt_in("par%d" % l, [128, PCOLS])
        self.fnw_d = dt_in("fnw", [128, 8])
        self.cos_d = dt_in("cos", [128, self.nsub * 32])
        self.sin_d = dt_in("sin", [128, self.nsub * 32])
        self.rdm_d = dt_in("rdm", [128, 512])
        self.rqd_d = dt_in("rqd", [128, 256])
        self.rcd_d = dt_in("rcd", [128, 2])
        self.rkd_d = dt_in("rkd", [128, 4])
        self.dbg_out = {}
        for name, shape in self.dbg:
            self.dbg_out[name] = nc.dram_tensor("dbg_" + name, list(shape), F32, kind="ExternalOutput").ap()
        self.scr = {}
        for l in self.layers:
            for mat, slabs, kcn in (('in', IN_SLABS, 8), ('out', [(0, 512), (512, 512)], 8), ('up', UP_SLABS, 8),
                                    ('down', [(m * 128, 128) for m in range(8)], 22)):
                for s, (c0, cw) in enumerate(slabs):
                    t = nc.dram_tensor("scr_%d_%s_%d" % (l, mat, s), [128, kcn, cw], BF16).ap()
                    self.scr[(l, mat, s)] = (T(t, 'scr'), c0, cw, kcn)
        self.alloc_all(nslots)
        self.build()
        fw.emit()

    def alloc_all(self, nslots):
        fw = self.fw
        sb = fw.sb
        self.ident_f = sb([128, 128], F32)
        self.ident_b = sb([128, 128], BF16)
        self.U = sb([128, 128], F32)
        self.ones_f = sb([128, 128], F32)
        self.ones_b = sb([128, 128], BF16)
        self.cosT = sb([128, self.nsub * 32], F32)
        self.sinT = sb([128, self.nsub * 32], F32)
        self.rdm = sb([128, 512], F32)
        self.rqd = sb([128, 256], F32)
        self.rcd = sb([128, 2], F32)
        self.rkd = sb([128, 4], F32)
        self.fnw = sb([128, 8], F32)
        self.par = {l: sb([128, PCOLS], F32) for l in self.layers}
        self.aneg = {l: sb([128, 8], F32) for l in self.layers}
        self.DI = {l: sb([128, 8 * 128], BF16) for l in self.layers}
        self.cneg = {l: sb([128, 4], F32) for l in self.layers}
        self.wab = {l: sb([128, 256], BF16) for l in self.layers}
        self.wxb = {l: sb([128, 256], BF16) for l in self.layers}
        self.R = {l: sb([128, 128], F32) for l in self.layers}
        self.Rb = {l: sb([128, 128], BF16) for l in self.layers}
        self.S = {l: sb([128, 512], F32) for l in self.layers}
        self.Sb = {l: sb([128, 512], BF16) for l in self.layers}
        self.hst = {l: sb([128, 2], F32) for l in self.layers}
        self.shalo = {l: sb([128, 8 * 3], F32) for l in self.layers}
        self.lhalo = {l: sb([128, 2 * 3], F32) for l in self.layers}
        self.fhalo = {l: [sb([128, 88 * 2], F32) for _ in range(2)] for l in self.layers}
        self.xin = [sb([128, D], F32) for _ in range(2)]
        self.xin_i = 0
        self.ost = [sb([128, D], F32) for _ in range(2)]
        self.ost_i = 0
        self.xF = sb([128, 8 * TT], F32)
        self.xFc = [T(self.xF.t, 'xF%d' % m) for m in range(8)]
        self.h = sb([128, 8 * TT], BF16)
        self.yF = sb([128, 8 * TT], BF16)
        self.hc = [T(self.h.t, 'h%d' % m) for m in range(8)]
        self.yFc = [T(self.yF.t, 'yF%d' % m) for m in range(8)]
        self.qkT = sb([128, 4 * TT], BF16)
        self.qz = sb([128, 4 * TT], BF16)
        self.qdz = sb([128, 4 * TT], BF16)
        self.wslot = [sb([128, SLOTW], BF16) for _ in range(nslots)]
        self.wslot_i = 0
        self.poolF = [T(sb([128, 515], F32).t, 'F%d' % i, 'F') for i in range(14)]
        self.poolH = [T(sb([128, 512], BF16).t, 'H%d' % i, 'H') for i in range(34)]
        self.poolS = [T(sb([128, 32], F32).t, 'S%d' % i, 'S') for i in range(20)]
        self.poolM = [T(sb([128, 128], BF16).t, 'M%d' % i, 'M') for i in range(16)]
        self.poolL = [T(sb([128, 128], F32).t, 'L%d' % i, 'L') for i in range(10)]
        self.pools = {'F': self.poolF, 'H': self.poolH, 'S': self.poolS, 'M': self.poolM, 'L': self.poolL}
        self.psb = [fw.ps([128, 512], F32) for _ in range(8)]
        self.ps_i = 0

    def get(self, p):
        lst = self.pools[p]
        assert lst, "pool %s empty" % p
        return lst.pop(0)

    def free(self, *ts):
        for t in ts:
            self.pools[t.pool].append(t)

    def nps(self):
        t = self.psb[self.ps_i]
        self.ps_i = (self.ps_i + 1) % 8
        return t

    def mm(self, out_ap, lhsT, rhs, start, stop, reads, writes, inc=None):
        if inc is None:
            inc = stop
        self.fw.op('pe', lambda t: t.matmul(out_ap, lhsT, rhs, start=start, stop=stop), reads, writes, inc=inc)

    def tr(self, out_ap, in_ap, ident_ap, reads, writes, inc=True):
        self.fw.op('pe', lambda t: t.transpose(out_ap, in_ap, ident_ap), reads, writes, inc=inc)

    def act(self, out_ap, in_ap, func, reads, writes, **kw):
        self.fw.op('act', lambda a: a.activation(out=out_ap, in_=in_ap, func=func, **kw), reads, writes)

    def tt(self, out_ap, in0, in1, op, reads, writes, eng='dve'):
        self.fw.op(eng, lambda v: v.tensor_tensor(out=out_ap, in0=in0, in1=in1, op=op), reads, writes)

    def ts(self, out_ap, in0, s1, s2, op0, op1, reads, writes, eng='dve'):
        if op1 is None:
            self.fw.op(eng, lambda v: v.tensor_scalar(out=out_ap, in0=in0, scalar1=s1, scalar2=None, op0=op0), reads, writes)
        else:
            self.fw.op(eng, lambda v: v.tensor_scalar(out=out_ap, in0=in0, scalar1=s1, scalar2=s2, op0=op0, op1=op1), reads, writes)

    def stt(self, out_ap, in0, scalar, in1, op0, op1, reads, writes):
        self.fw.op('dve', lambda v: v.scalar_tensor_tensor(out=out_ap, in0=in0, scalar=scalar, in1=in1, op0=op0, op1=op1), reads, writes)

    def cp(self, eng, out_ap, in_ap, reads, writes):
        if eng == 'act':
            self.fw.op('act', lambda a: a.copy(out=out_ap, in_=in_ap), reads, writes)
        else:
            self.fw.op(eng, lambda v: v.tensor_copy(out=out_ap, in_=in_ap), reads, writes)

    def memset(self, eng, t, ap, val):
        self.fw.op(eng, lambda v: v.memset(ap, val), [], [t])

    def P(self, l, name, a=None, b=None):
        o, n = POFF[name]
        if a is None:
            return self.par[l][:, o:o + n]
        return self.par[l][:, o + a:o + b]

    def dump(self, name, t, ap):
        if name in self.dbg_out:
            self.fw.dma('pool', self.dbg_out[name], ap, reads=[t])

    def build_wsched(self):
        sched = []
        for seq in range(self.nseq):
            for ti in range(self.ntile):
                for l in self.layers:
                    for s in range(6):
                        sched.append((l, 'in', s))
                    for s in range(2):
                        sched.append((l, 'out', s))
                    for s in range(12):
                        sched.append((l, 'up', s))
                    for s in range(8):
                        sched.append((l, 'down', s))
        self.wsched = sched
        self.w_loaded = 0
        self.w_used = 0
        self.w_slotof = {}

    def wload(self, key):
        assert self.wsched[self.w_used] == key, (self.wsched[self.w_used], key)
        ns = len(self.wslot)
        while self.w_loaded < len(self.wsched) and self.w_loaded < self.w_used + ns - 1:
            k = self.wsched[self.w_loaded]
            st, c0, cw, kcn = self.scr[k]
            slot = self.wslot[self.w_loaded % ns]
            self.fw.dma('sp', slot[:, 0:kcn * cw], st.t.rearrange("p k c -> p (k c)"), reads=[st], writes=[slot])
            self.w_loaded += 1
        slot = self.wslot[self.w_used % ns]
        self.w_used += 1
        return slot, self.scr[key][2]

    def build(self):
        fw = self.fw
        self.memset('pool', self.ones_f, self.ones_f[:], 1.0)
        self.memset('pool', self.qz, self.qz[:], 0.0)
        self.memset('pool', self.qdz, self.qdz[:], 0.0)
        self.memset('pool', self.ones_b, self.ones_b[:], 1.0)
        self.memset('pool', self.U, self.U[:], 1.0)
        fw.op('pool', lambda g: g.affine_select(out=self.U[:], in_=self.U[:], pattern=[[1, 128]], compare_op=ALU.is_ge,
                                                fill=0.0, base=0, channel_multiplier=-1), [self.U], [self.U])
        self.memset('pool', self.ident_f, self.ident_f[:], 1.0)
        fw.op('pool', lambda g: g.affine_select(out=self.ident_f[:], in_=self.ident_f[:], pattern=[[1, 128]], compare_op=ALU.is_equal,
                                                fill=0.0, base=0, channel_multiplier=-1), [self.ident_f], [self.ident_f])
        self.cp('dve', self.ident_b[:], self.ident_f[:], [self.ident_f], [self.ident_b])
        for t, d in ((self.cosT, self.cos_d), (self.sinT, self.sin_d), (self.rdm, self.rdm_d), (self.rqd, self.rqd_d),
                     (self.rcd, self.rcd_d), (self.rkd, self.rkd_d), (self.fnw, self.fnw_d)):
            fw.dma('sp', t[:], d, writes=[t])
        for l in self.layers:
            fw.dma('sp', self.par[l][:], self.wd[(l, 'par')], writes=[self.par[l]])
        for l in self.layers:
            for mat, n in (('in', 6), ('out', 2), ('up', 12), ('down', 8)):
                for s in range(n):
                    st, c0, cw, kcn = self.scr[(l, mat, s)]
                    src = self.wd[(l, mat)][:, c0:c0 + cw].rearrange("(k p) c -> p k c", p=128)
                    fw.dma('pool', st.t, src, writes=[st])
        for l in self.layers:
            par = self.par[l]
            self.act(self.aneg[l][:], self.P(l, 'alog'), AF.Exp, [par], [self.aneg[l]])
            self.ts(self.aneg[l][:], self.aneg[l][:], -1.0, None, ALU.mult, None, [self.aneg[l]], [self.aneg[l]])
            for hd in range(8):
                self.ts(self.DI[l][:, hd * 128:(hd + 1) * 128], self.ident_f[:], self.P(l, 'dsk', hd, hd + 1), None, ALU.mult, None,
                        [self.ident_f, par], [self.DI[l]])
            tmp = self.get('S')
            self.act(tmp[:, 0:2], self.P(l, 'lam'), AF.Exp, [par], [tmp], scale=-1.0)
            self.act(tmp[:, 0:2], tmp[:, 0:2], AF.Ln, [tmp], [tmp], bias=1.0)
            self.ts(self.cneg[l][:, 0:2], tmp[:, 0:2], -8.0, None, ALU.mult, None, [tmp], [self.cneg[l]])
            self.ts(self.cneg[l][:, 2:4], tmp[:, 0:2], -16.0, None, ALU.mult, None, [tmp], [self.cneg[l]])
            self.free(tmp)
            self.cp('dve', self.wab[l][:], self.P(l, 'wa'), [par], [self.wab[l]])
            self.cp('dve', self.wxb[l][:], self.P(l, 'wx'), [par], [self.wxb[l]])
        self.build_wsched()
        for seq in range(self.nseq):
            for l in self.layers:
                for t in (self.R[l], self.Rb[l], self.S[l], self.Sb[l], self.hst[l], self.shalo[l], self.lhalo[l], self.fhalo[l][0], self.fhalo[l][1]):
                    self.memset('pool', t, t[:], 0.0)
            for ti in range(self.ntile):
                self.load_x(seq, ti)
                try:
                    for l in self.layers:
                        self.layer(l, ti)
                except StopBuild:
                    pass
                self.store_out(seq, ti)
        fw.wait_all_dma('sp')

    def load_x(self, seq, ti):
        fw = self.fw
        for st in range(NST):
            xin = self.xin[self.xin_i]
            self.xin_i ^= 1
            t0 = ti * TT + st * 128
            fw.dma('sp', xin[:], self.x[seq, t0:t0 + 128, :], writes=[xin])
            for half in range(2):
                ps = self.nps()
                for k4 in range(4):
                    kc = half * 4 + k4
                    self.tr(ps[:, k4 * 128:(k4 + 1) * 128], xin[:, kc * 128:(kc + 1) * 128], self.ident_f[:], [xin, self.ident_f], [ps], inc=(k4 == 3))
                dst = self.xF[:, half * 4 * TT:(half + 1) * 4 * TT].rearrange("p (k t) -> p k t", k=4)[:, :, st * 128:(st + 1) * 128]
                self.cp('act', dst, ps[:, :].rearrange("p (k t) -> p k t", k=4), [ps], self.xFc[half * 4:(half + 1) * 4])

    def rms_rstd(self, src):
        sq = [self.get('H') for _ in range(8)]
        for kc in range(8):
            self.act(sq[kc][:, :], src[:, kc * TT:(kc + 1) * TT], AF.Square, [self.xFc[kc]], [sq[kc]])
        ps = self.nps()
        for kc in range(8):
            self.mm(ps[:, :], self.ones_b[:], sq[kc][:, :], kc == 0, kc == 7, [self.ones_b, sq[kc]], [ps])
        self.free(*sq)
        r = self.get('F')
        self.act(r[:, 0:TT], ps[:, :], AF.Ln, [ps], [r], scale=1.0 / D, bias=self.epsT[:, 0:1])
        self.act(r[:, 0:TT], r[:, 0:TT], AF.Exp, [r], [r], scale=-0.5)
        return r

    def norm_to_h(self, l, wname):
        r = self.rms_rstd(self.xF)
        for kc in range(8):
            self.stt(self.h[:, kc * TT:(kc + 1) * TT], self.xF[:, kc * TT:(kc + 1) * TT], self.P(l, wname, kc, kc + 1), r[:, 0:TT],
                     ALU.mult, ALU.mult, [self.xFc[kc], self.par[l], r], [self.hc[kc]])
        self.free(r)

    def store_out(self, seq, ti):
        fw = self.fw
        if self.final:
            r = self.rms_rstd(self.xF)
            yo = [self.get('F') for _ in range(8)]
            for kc in range(8):
                self.stt(yo[kc][:, 0:TT], self.xF[:, kc * TT:(kc + 1) * TT], self.fnw[:, kc:kc + 1], r[:, 0:TT], ALU.mult, ALU.mult,
                         [self.xFc[kc], self.fnw, r], [yo[kc]])
            self.free(r)
            srcs = yo
            get = lambda kc, st: yo[kc][:, st * 128:(st + 1) * 128]
        else:
            srcs = list(self.xFc)
            get = lambda kc, st: self.xF[:, kc * TT + st * 128: kc * TT + (st + 1) * 128]
        for st in range(NST):
            ost = self.ost[self.ost_i]
            self.ost_i ^= 1
            for half in range(2):
                ps = self.nps()
                for k4 in range(4):
                    kc = half * 4 + k4
                    self.tr(ps[:, k4 * 128:(k4 + 1) * 128], get(kc, st), self.ident_f[:], [srcs[kc], self.ident_f], [ps], inc=(k4 == 3))
                self.cp('act', ost[:, half * 512:(half + 1) * 512], ps[:, :], [ps], [ost])
            t0 = ti * TT + st * 128
            fw.dma('act', self.y[seq, t0:t0 + 128, :], ost[:], reads=[ost])
        if self.final:
            self.free(*yo)

    def ffn_fin(self, accs, hid):
        au, av = accs
        self.act(au[:, 0:TT], au[:, 0:TT], AF.Gelu_apprx_tanh, [au], [au])
        o = self.get('H')
        self.tt(o[:, :], au[:, 0:TT], av[:, 0:TT], ALU.mult, [au, av], [o], eng='pool')
        self.free(au, av)
        hid.append(o)

    def layer(self, l, ti):
        par = self.par[l]
        h = self.h
        hk = lambda kc, a=0, b=TT: h[:, kc * TT + a: kc * TT + b]
        sub0 = ti * NST
        self.ck(0)
        self.norm_to_h(l, 'n1w')
        self.ck(1)
        W, cw = self.wload((l, 'in', 0))
        qk_TM = [self.get('H') for _ in range(NST)]
        for st in range(NST):
            ps = self.nps()
            for kc in range(8):
                self.mm(ps[:, :], hk(kc, st * 128, (st + 1) * 128), W[:, kc * cw: kc * cw + 512], kc == 0, kc == 7, [self.hc[kc], W], [ps])
            self.ck(101)
            v4 = ps[:, :].rearrange("p (h t d) -> p h t d", h=8, t=2)
            x1, x2 = v4[:, :, 0, :], v4[:, :, 1, :]
            o4 = qk_TM[st][:, :].rearrange("p (h t d) -> p h t d", h=8, t=2)
            sub = sub0 + st
            cosb = self.cosT[:, sub * 32:(sub + 1) * 32].unsqueeze(1).to_broadcast([128, 8, 32])
            sinb = self.sinT[:, sub * 32:(sub + 1) * 32].unsqueeze(1).to_broadcast([128, 8, 32])
            t1, t2 = self.get('F'), self.get('F')
            a1 = t1[:, 0:256].rearrange("p (h d) -> p h d", h=8)
            a2 = t2[:, 0:256].rearrange("p (h d) -> p h d", h=8)
            self.tt(a1, x1, cosb, ALU.mult, [ps, self.cosT], [t1])
            self.tt(a2, x2, sinb, ALU.mult, [ps, self.sinT], [t2])
            self.tt(o4[:, :, 0, :], a1, a2, ALU.subtract, [t1, t2], [qk_TM[st]])
            self.tt(a1, x1, sinb, ALU.mult, [ps, self.sinT], [t1])
            self.tt(a2, x2, cosb, ALU.mult, [ps, self.cosT], [t2])
            self.tt(o4[:, :, 1, :], a1, a2, ALU.add, [t1, t2], [qk_TM[st]])
            self.free(t1, t2)
            self.ck(102)
            pt = self.nps()
            ptb = pt[:, :].bitcast(BF16)
            for c in range(4):
                self.tr(ptb[:, c * 128:(c + 1) * 128], qk_TM[st][:, c * 128:(c + 1) * 128], self.ident_b[:], [qk_TM[st], self.ident_b], [pt], inc=(c == 3))
            self.ck(103)
            dst = self.qkT[:, 2 * TT:4 * TT].rearrange("p (c t) -> p c t", c=2)[:, :, st * 128:(st + 1) * 128]
            self.cp('act', dst, ptb[:, 256:512].rearrange("p (c t) -> p c t", c=2), [pt], [self.qkT])
            self.ck(104)
            for hh in range(2):
                pr = slice(hh * 64, (hh + 1) * 64)
                dz = self.qz[pr, :].rearrange("p (c h t) -> p c h t", c=2, h=2)[:, :, hh, st * 128:(st + 1) * 128]
                ddz = self.qdz[pr, :].rearrange("p (c h t) -> p c h t", c=2, h=2)[:, :, hh, st * 128:(st + 1) * 128]
                self.cp('act', dz, ptb[pr, 0:256].rearrange("p (c t) -> p c t", c=2), [pt], [self.qz])
                self.tt(ddz, dz, self.rqd[pr, :].rearrange("p (c t) -> p c t", c=2), ALU.mult, [self.qz, self.rqd], [self.qdz])
        self.ck(2)
        W, cw = self.wload((l, 'in', 1))
        v_TM = [self.get('H') for _ in range(NST)]
        sg = [self.get('F') for _ in range(NST)]
        for st in range(NST):
            ps = self.nps()
            for kc in range(8):
                self.mm(ps[:, :], hk(kc, st * 128, (st + 1) * 128), W[:, kc * cw: kc * cw + 512], kc == 0, kc == 7, [self.hc[kc], W], [ps])
            self.cp('act', v_TM[st][:, 0:256], ps[:, 0:256], [ps], [v_TM[st]])
            self.tt(v_TM[st][:, 256:512].rearrange("p (h d) -> p h d", h=4), ps[:, 0:256].rearrange("p (h d) -> p h d", h=4),
                    self.rkd[:, 0:4].unsqueeze(2).to_broadcast([128, 4, 64]), ALU.mult, [ps, self.rkd], [v_TM[st]])
            self.act(sg[st][:, 0:256], ps[:, 256:512], AF.Silu, [ps], [sg[st]])
        self.ck(3)
        R, Rb = self.R[l], self.Rb[l]
        for st in range(NST):
            sc = slice(st * 128, (st + 1) * 128)
            ps = self.nps()
            for hd in range(4):
                c, hh = hd // 2, hd % 2
                pr = slice(hh * 64, (hh + 1) * 64)
                self.mm(ps[:, hd * 128:(hd + 1) * 128], self.qkT[:, (2 + c) * TT + st * 128:(2 + c) * TT + (st + 1) * 128],
                        self.qz[:, hd * TT + st * 128: hd * TT + (st + 1) * 128], True, True, [self.qkT, self.qz], [ps], inc=(hd == 3))
            self.ck(201)
            PT = self.get('H')
            self.tt(PT[:, :], ps[:, :], self.rdm[:, :], ALU.mult, [ps, self.rdm], [PT])
            self.ck(202)
            py = self.nps()
            for hd in range(4):
                c, hh = hd // 2, hd % 2
                pr = slice(hh * 64, (hh + 1) * 64)
                self.mm(py[:, hd * 64:(hd + 1) * 64], PT[:, hd * 128:(hd + 1) * 128], v_TM[st][:, hd * 64:(hd + 1) * 64], True, False,
                        [PT, v_TM[st]], [py], inc=False)
                self.mm(py[:, hd * 64:(hd + 1) * 64], self.qdz[:, hd * TT + st * 128: hd * TT + (st + 1) * 128], Rb[:, c * 64:(c + 1) * 64],
                        False, True, [self.qdz, Rb], [py], inc=(hd == 3))
            self.free(PT)
            self.ck(203)
            pk = self.nps()
            for c in range(2):
                self.mm(pk[:, c * 128:(c + 1) * 128], qk_TM[st][:, 256 + c * 128: 256 + (c + 1) * 128], v_TM[st][:, 256 + c * 128: 256 + (c + 1) * 128],
                        True, True, [qk_TM[st], v_TM[st]], [pk], inc=(c == 1))
            self.ck(204)
            for c in range(2):
                for hh in range(2):
                    pr = slice(hh * 64, (hh + 1) * 64)
                    self.stt(R[pr, c * 64:(c + 1) * 64], R[pr, c * 64:(c + 1) * 64], self.rcd[pr, c:c + 1],
                             pk[pr, c * 128 + hh * 64: c * 128 + (hh + 1) * 64], ALU.mult, ALU.add, [R, self.rcd, pk], [R])
            self.ck(205)
            self.cp('act', Rb[:, :], R[:, :], [R], [Rb])
            self.ck(206)
            sm = self.get('S')
            y3 = py[:, 0:256].rearrange("p (h d) -> p h d", h=4)
            self.fw.op('dve', lambda v, sm=sm, y3=y3: v.tensor_reduce(out=sm[:, 0:4], in_=y3, axis=AX.X, op=ALU.add), [py], [sm])
            self.ck(207)
            ysq = self.get('F')
            self.act(ysq[:, 0:256], py[:, 0:256], AF.Square, [py], [ysq])
            self.fw.op('dve', lambda v, sm=sm, ysq=ysq: v.tensor_reduce(out=sm[:, 4:8], in_=ysq[:, 0:256].rearrange("p (h d) -> p h d", h=4),
                                                                      axis=AX.X, op=ALU.add), [ysq], [sm])
            self.ts(sm[:, 0:4], sm[:, 0:4], 1.0 / 64, None, ALU.mult, None, [sm], [sm])
            self.tt(sm[:, 8:12], sm[:, 0:4], sm[:, 0:4], ALU.mult, [sm], [sm])
            self.stt(sm[:, 4:8], sm[:, 4:8], 1.0 / 64, sm[:, 8:12], ALU.mult, ALU.subtract, [sm], [sm])
            self.act(sm[:, 4:8], sm[:, 4:8], AF.Ln, [sm], [sm], bias=self.epsT[:, 0:1])
            self.act(sm[:, 4:8], sm[:, 4:8], AF.Exp, [sm], [sm], scale=-0.5)
            self.ck(208)
            yc = ysq
            yc3 = yc[:, 0:256].rearrange("p (h d) -> p h d", h=4)
            self.tt(yc3, y3, sm[:, 0:4].unsqueeze(2).to_broadcast([128, 4, 64]), ALU.subtract, [py, sm], [yc])
            self.tt(yc3, yc3, sm[:, 4:8].unsqueeze(2).to_broadcast([128, 4, 64]), ALU.mult, [yc, sm], [yc])
            self.ck(209)
            yb = self.get('H')
            self.tt(yb[:, 0:256], yc[:, 0:256], sg[st][:, 0:256], ALU.mult, [yc, sg[st]], [yb])
            self.free(sm, ysq)
            pt = self.nps()
            ptb = pt[:, :].bitcast(BF16)
            for c in range(2):
                self.tr(ptb[:, c * 128:(c + 1) * 128], yb[:, c * 128:(c + 1) * 128], self.ident_b[:], [yb, self.ident_b], [pt], inc=(c == 1))
            self.free(yb)
            dst = self.yF[:, 0:2 * TT].rearrange("p (c t) -> p c t", c=2)[:, :, sc]
            self.cp('act', dst, ptb[:, 0:256].rearrange("p (c t) -> p c t", c=2), [pt], self.yFc[0:2])
        self.free(*qk_TM)
        self.free(*v_TM)
        self.free(*sg)
        self.ck(4)
        W, cw = self.wload((l, 'in', 2))
        pdt = self.nps()
        for st in range(NST):
            for kc in range(8):
                self.mm(pdt[:, st * 8:(st + 1) * 8], hk(kc, st * 128, (st + 1) * 128), W[:, kc * cw: kc * cw + 8], kc == 0, kc == 7,
                        [self.hc[kc], W], [pdt], inc=(kc == 7 and st == NST - 1))
        Sx, Sa, Sdt, SdA, Sln, Scum, Stot, Sncl, Secum, Sdec, Swg = [self.get('S') for _ in range(11)]
        b3 = lambda ap: ap.unsqueeze(1).to_broadcast([128, 4, 8])
        v3 = lambda t: t[:, 0:32].rearrange("p (s h) -> p s h", s=4)
        self.tt(v3(Sx), pdt[:, 0:32].rearrange("p (s h) -> p s h", s=4), b3(self.P(l, 'dtb')), ALU.add, [pdt, par], [Sx])
        self.act(Sa[:, 0:32], Sx[:, 0:32], AF.Abs, [Sx], [Sa])
        self.act(Sa[:, 0:32], Sa[:, 0:32], AF.Exp, [Sa], [Sa], scale=-1.0)
        self.act(Sa[:, 0:32], Sa[:, 0:32], AF.Ln, [Sa], [Sa], bias=1.0)
        self.stt(Sdt[:, 0:32], Sx[:, 0:32], 0.0, Sa[:, 0:32], ALU.max, ALU.add, [Sx, Sa], [Sdt])
        self.tt(v3(SdA), v3(Sdt), b3(self.aneg[l][:, 0:8]), ALU.mult, [Sdt, self.aneg[l]], [SdA])
        self.act(Sln[:, 0:32], Sdt[:, 0:32], AF.Ln, [Sdt], [Sln])
        pc = self.nps()
        self.mm(pc[:, 0:32], self.U[:], SdA[:, 0:32], True, True, [self.U, SdA], [pc], inc=False)
        self.mm(pc[:, 32:64], self.ones_f[:], SdA[:, 0:32], True, True, [self.ones_f, SdA], [pc])
        self.cp('act', Scum[:, 0:32], pc[:, 0:32], [pc], [Scum])
        self.cp('act', Stot[:, 0:32], pc[:, 32:64], [pc], [Stot])
        self.act(Secum[:, 0:32], Scum[:, 0:32], AF.Exp, [Scum], [Secum])
        self.act(Sdec[:, 0:32], Stot[:, 0:32], AF.Exp, [Stot], [Sdec])
        self.tt(Swg[:, 0:32], Stot[:, 0:32], Scum[:, 0:32], ALU.subtract, [Stot, Scum], [Swg])
        self.act(Swg[:, 0:32], Swg[:, 0:32], AF.Exp, [Swg], [Swg])
        self.tt(Swg[:, 0:32], Swg[:, 0:32], Sdt[:, 0:32], ALU.mult, [Swg, Sdt], [Swg])
        self.free(Sx, Sa, Stot, Sncl)
        self.ck(5)
        gg = []
        for c in range(2):
            ps = self.nps()
            for kc in range(8):
                self.mm(ps[:, :], W[:, kc * cw + 8 + c * 128: kc * cw + 8 + (c + 1) * 128], hk(kc), kc == 0, kc == 7, [W, self.hc[kc]], [ps])
            g_ = self.get('F')
            self.act(g_[:, 0:TT], ps[:, :], AF.Gelu_apprx_tanh, [ps], [g_])
            gg.append(g_)
        for c in range(2):
            ps = self.nps()
            for kc in range(8):
                self.mm(ps[:, :], W[:, kc * cw + 264 + c * 128: kc * cw + 264 + (c + 1) * 128], hk(kc), kc == 0, kc == 7, [W, self.hc[kc]], [ps])
            raw = self.get('F')
            halo = self.lhalo[l]
            self.cp('pool', raw[:, 0:3], halo[:, c * 3:(c + 1) * 3], [halo], [raw])
            self.cp('act', raw[:, 3:515], ps[:, :], [ps], [raw])
            xc = self.get('F')
            lcw = lambda k: self.P(l, 'lcw', c * 4 + k, c * 4 + k + 1)
            self.act(xc[:, 0:TT], raw[:, 3:515], AF.Identity, [raw, par], [xc], scale=lcw(3), bias=self.P(l, 'lcb', c, c + 1))
            for k in (2, 1, 0):
                self.stt(xc[:, 0:TT], raw[:, k:k + TT], lcw(k), xc[:, 0:TT], ALU.mult, ALU.add, [raw, par, xc], [xc])
            self.cp('pool', halo[:, c * 3:(c + 1) * 3], raw[:, 512:515], [raw], [halo])
            self.free(raw)
            xcb = self.get('H')
            self.cp('act', xcb[:, :], xc[:, 0:TT], [xc], [xcb])
            pr_ = self.nps()
            self.mm(pr_[:, :], self.wab[l][:, c * 128:(c + 1) * 128], xcb[:, :], True, True, [self.wab[l], xcb], [pr_])
            pi_ = self.nps()
            self.mm(pi_[:, :], self.wxb[l][:, c * 128:(c + 1) * 128], xcb[:, :], True, True, [self.wxb[l], xcb], [pi_])
            self.free(xcb)
            r_, i_, a_ = self.get('F'), self.get('F'), self.get('F')
            self.act(r_[:, 0:TT], pr_[:, :], AF.Sigmoid, [pr_, par], [r_], bias=self.P(l, 'ba', c, c + 1))
            self.act(i_[:, 0:TT], pi_[:, :], AF.Sigmoid, [pi_, par], [i_], bias=self.P(l, 'bx', c, c + 1))
            self.act(a_[:, 0:TT], r_[:, 0:TT], AF.Exp, [r_, self.cneg[l]], [a_], scale=self.cneg[l][:, c:c + 1])
            self.act(r_[:, 0:TT], r_[:, 0:TT], AF.Exp, [r_, self.cneg[l]], [r_], scale=self.cneg[l][:, 2 + c:3 + c])
            self.act(r_[:, 0:TT], r_[:, 0:TT], AF.Sqrt, [r_], [r_], scale=-1.0, bias=1.0)
            self.tt(i_[:, 0:TT], i_[:, 0:TT], xc[:, 0:TT], ALU.mult, [i_, xc], [i_])
            self.tt(i_[:, 0:TT], i_[:, 0:TT], r_[:, 0:TT], ALU.mult, [i_, r_], [i_])
            hs = self.hst[l]
            self.fw.op('dve', lambda v, xc=xc, a_=a_, i_=i_, hs=hs, c=c: v.tensor_tensor_scan(
                out=xc[:, 0:TT], data0=a_[:, 0:TT], data1=i_[:, 0:TT], initial=hs[:, c:c + 1], op0=ALU.mult, op1=ALU.add),
                [a_, i_, hs], [xc])
            self.cp('dve', hs[:, c:c + 1], xc[:, TT - 1:TT], [xc], [hs])
            self.tt(self.yF[:, (6 + c) * TT:(7 + c) * TT], xc[:, 0:TT], gg[c][:, 0:TT], ALU.mult, [xc, gg[c]], [self.yFc[6 + c]])
            self.free(r_, i_, a_, xc, gg[c])
        self.ck(6)
        W, cw = self.wload((l, 'in', 3))
        sz = [self.get('F') for _ in range(NST)]
        for st in range(NST):
            ps = self.nps()
            for kc in range(8):
                self.mm(ps[:, :], hk(kc, st * 128, (st + 1) * 128), W[:, kc * cw: kc * cw + 512], kc == 0, kc == 7, [self.hc[kc], W], [ps])
            self.act(sz[st][:, 0:TT], ps[:, :], AF.Silu, [ps], [sz[st]])
        self.ck(7)
        xbcF = []
        for slab in (4, 5):
            W, cw = self.wload((l, 'in', slab))
            for m in range(4):
                cidx = (slab - 4) * 4 + m
                ps = self.nps()
                for kc in range(8):
                    self.mm(ps[:, :], W[:, kc * cw + m * 128: kc * cw + (m + 1) * 128], hk(kc), kc == 0, kc == 7, [W, self.hc[kc]], [ps])
                raw = self.get('F')
                halo = self.shalo[l]
                self.cp('pool', raw[:, 0:3], halo[:, cidx * 3:(cidx + 1) * 3], [halo], [raw])
                self.cp('act', raw[:, 3:515], ps[:, :], [ps], [raw])
                acc = self.get('F')
                scw = lambda k: self.P(l, 'scw', cidx * 4 + k, cidx * 4 + k + 1)
                self.act(acc[:, 0:TT], raw[:, 3:515], AF.Identity, [raw, par], [acc], scale=scw(3), bias=self.P(l, 'scb', cidx, cidx + 1))
                for k in (2, 1, 0):
                    self.stt(acc[:, 0:TT], raw[:, k:k + TT], scw(k), acc[:, 0:TT], ALU.mult, ALU.add, [raw, par, acc], [acc])
                self.cp('pool', halo[:, cidx * 3:(cidx + 1) * 3], raw[:, 512:515], [raw], [halo])
                self.free(raw)
                o = self.get('H')
                self.act(o[:, :], acc[:, 0:TT], AF.Silu, [acc], [o])
                self.free(acc)
                xbcF.append(o)
        xsF, BF, CF = xbcF[0:4], xbcF[4:6], xbcF[6:8]
        self.ck(8)
        S, Sb = self.S[l], self.Sb[l]

        def ssd_A(st):
            sc = slice(st * 128, (st + 1) * 128)
            xs_TM = self.get('H')
            pt = self.nps()
            ptb = pt[:, :].bitcast(BF16)
            for c in range(4):
                self.tr(ptb[:, c * 128:(c + 1) * 128], xsF[c][:, sc], self.ident_b[:], [xsF[c], self.ident_b], [pt], inc=(c == 3))
            self.cp('act', xs_TM[:, :], ptb[:, 0:512], [pt], [xs_TM])
            B_TM = self.get('H')
            pt = self.nps()
            ptb = pt[:, :].bitcast(BF16)
            for g in range(2):
                self.tr(ptb[:, g * 128:(g + 1) * 128], BF[g][:, sc], self.ident_b[:], [BF[g], self.ident_b], [pt], inc=(g == 1))
            self.cp('act', B_TM[:, 0:256], ptb[:, 0:256], [pt], [B_TM])
            pcb = self.nps()
            for g in range(2):
                self.mm(pcb[:, g * 128:(g + 1) * 128], BF[g][:, sc], CF[g][:, sc], True, True, [BF[g], CF[g]], [pcb], inc=(g == 1))
            CBm = [self.get('L') for _ in range(2)]
            for g in range(2):
                self.tt(CBm[g][:, :], pcb[:, g * 128:(g + 1) * 128], self.U[:, :], ALU.mult, [pcb, self.U], [CBm[g]])
            MT = []
            Lrs = []
            pqs = []
            for hq in range(2):
                pq = self.nps()
                for j in range(4):
                    hd = hq * 4 + j
                    col = st * 8 + hd
                    self.mm(pq[:, j * 128:(j + 1) * 128], SdA[:, col:col + 1].to_broadcast([128, 128]), self.U[:], True, True,
                            [SdA, self.U], [pq], inc=(j == 3))
                pqs.append(pq)
            for hd in range(8):
                col = st * 8 + hd
                pq, j = pqs[hd // 4], hd % 4
                Lr = self.get('L')
                self.ts(Lr[:, :], pq[:, j * 128:(j + 1) * 128], Scum[:, col:col + 1], 0.0, ALU.subtract, ALU.min, [pq, Scum], [Lr])
                Lrs.append(Lr)
            for hd in range(8):
                col = st * 8 + hd
                self.act(Lrs[hd][:, :], Lrs[hd][:, :], AF.Exp, [Lrs[hd], Sln], [Lrs[hd]], bias=Sln[:, col:col + 1])
            for hd in range(8):
                m_ = self.get('M')
                self.tt(m_[:, :], Lrs[hd][:, :], CBm[hd // 4][:, :], ALU.mult, [Lrs[hd], CBm[hd // 4]], [m_])
                MT.append(m_)
            self.free(*Lrs)
            self.free(*CBm)
            xw = self.get('H')
            self.tt(xw[:, :].rearrange("p (h d) -> p h d", h=8), xs_TM[:, :].rearrange("p (h d) -> p h d", h=8),
                    Swg[:, st * 8:(st + 1) * 8].unsqueeze(2).to_broadcast([128, 8, 64]), ALU.mult, [xs_TM, Swg], [xw])
            return xs_TM, B_TM, MT, xw

        def ssd_B(st, xs_TM, B_TM, MT, xw):
            sc = slice(st * 128, (st + 1) * 128)
            pY = self.nps()
            for hd in range(8):
                hs_ = slice(hd * 64, (hd + 1) * 64)
                self.mm(pY[:, hs_], MT[hd][:, :], xs_TM[:, hs_], True, False, [MT[hd], xs_TM], [pY], inc=False)
                self.mm(pY[:, hs_], self.DI[l][:, hd * 128:(hd + 1) * 128], xs_TM[:, hs_], False, True, [self.DI[l], xs_TM], [pY], inc=(hd == 7))
            self.free(*MT)
            pO = self.nps()
            for g in range(2):
                self.mm(pO[:, g * 256:(g + 1) * 256], CF[g][:, sc], Sb[:, g * 256:(g + 1) * 256], True, True, [CF[g], Sb], [pO], inc=(g == 1))
            pS = self.nps()
            for g in range(2):
                self.mm(pS[:, g * 256:(g + 1) * 256], B_TM[:, g * 128:(g + 1) * 128], xw[:, g * 256:(g + 1) * 256], True, True, [B_TM, xw], [pS], inc=(g == 1))
            self.free(xw, B_TM, xs_TM)
            S3 = S[:, :].rearrange("p (h d) -> p h d", h=8)
            self.tt(S3, S3, Sdec[:, st * 8:(st + 1) * 8].unsqueeze(2).to_broadcast([128, 8, 64]), ALU.mult, [S, Sdec], [S])
            self.tt(S[:, :], S[:, :], pS[:, :], ALU.add, [S, pS], [S])
            self.cp('act', Sb[:, :], S[:, :], [S], [Sb])
            Y = self.get('F')
            self.tt(Y[:, 0:TT].rearrange("p (h d) -> p h d", h=8), pO[:, :].rearrange("p (h d) -> p h d", h=8),
                    Secum[:, st * 8:(st + 1) * 8].unsqueeze(2).to_broadcast([128, 8, 64]), ALU.mult, [pO, Secum], [Y])
            self.tt(Y[:, 0:TT], Y[:, 0:TT], pY[:, :], ALU.add, [Y, pY], [Y])
            self.tt(Y[:, 0:TT], Y[:, 0:TT], sz[st][:, 0:TT], ALU.mult, [Y, sz[st]], [Y])
            junk = self.get('F')
            ss = self.get('S')
            for g in range(2):
                self.fw.op('act', lambda a, junk=junk, Y=Y, ss=ss, g=g: a.activation(out=junk[:, g * 256:(g + 1) * 256], in_=Y[:, g * 256:(g + 1) * 256],
                                                                                  func=AF.Square, accum_out=ss[:, g:g + 1]), [Y], [junk, ss])
            self.free(junk)
            self.act(ss[:, 0:2], ss[:, 0:2], AF.Ln, [ss], [ss], scale=1.0 / 256, bias=self.epsT[:, 0:1])
            self.act(ss[:, 0:2], ss[:, 0:2], AF.Exp, [ss], [ss], scale=-0.5)
            yb = self.get('H')
            o_, n_ = POFF['snw']
            for g in range(2):
                self.stt(yb[:, g * 256:(g + 1) * 256], Y[:, g * 256:(g + 1) * 256], ss[:, g:g + 1], self.par[l][:, o_ + g * 256: o_ + (g + 1) * 256],
                         ALU.mult, ALU.mult, [Y, ss, par], [yb])
            self.free(Y, ss)
            pt = self.nps()
            ptb = pt[:, :].bitcast(BF16)
            for c in range(4):
                self.tr(ptb[:, c * 128:(c + 1) * 128], yb[:, c * 128:(c + 1) * 128], self.ident_b[:], [yb, self.ident_b], [pt], inc=(c == 3))
            self.free(yb)
            dst = self.yF[:, 2 * TT:6 * TT].rearrange("p (c t) -> p c t", c=4)[:, :, sc]
            self.cp('act', dst, ptb[:, 0:512].rearrange("p (c t) -> p c t", c=4), [pt], self.yFc[2:6])

        stA = ssd_A(0)
        for st in range(NST):
            nxt = ssd_A(st + 1) if st + 1 < NST else None
            ssd_B(st, *stA)
            stA = nxt
        self.free(*xbcF)
        self.free(*sz)
        self.free(Sdt, SdA, Sln, Scum, Secum, Sdec, Swg)
        self.dump('yF', self.yF, self.yF[:, :])
        self.ck(9)
        for m in range(8):
            if m % 4 == 0:
                W, cw = self.wload((l, 'out', m // 4))
            ps = self.nps()
            for kc in range(8):
                self.mm(ps[:, :], W[:, kc * cw + (m % 4) * 128: kc * cw + (m % 4 + 1) * 128], self.yF[:, kc * TT:(kc + 1) * TT], kc == 0, kc == 7,
                        [W, self.yFc[kc]], [ps])
            self.tt(self.xF[:, m * TT:(m + 1) * TT], ps[:, :], self.xF[:, m * TT:(m + 1) * TT], ALU.add, [ps, self.xFc[m]], [self.xFc[m]])
        self.dump('x1', self.xF, self.xF[:, :])
        self.ck(10)
        self.norm_to_h(l, 'n2w')
        hid = []
        pend = []
        fh = self.fhalo[l][ti % 2]
        fhn = self.fhalo[l][(ti + 1) % 2]
        for s in range(6):
            Wu, cwu = self.wload((l, 'up', 2 * s))
            Wv, cwv = self.wload((l, 'up', 2 * s + 1))
            for jj in range(cwu // 128):
                j = 4 * s + jj
                accs = []
                for (Wx, cwx, ci) in ((Wu, cwu, j), (Wv, cwv, 22 + j)):
                    ps = self.nps()
                    for kc in range(8):
                        self.mm(ps[:, :], Wx[:, kc * cwx + jj * 128: kc * cwx + (jj + 1) * 128], hk(kc), kc == 0, kc == 7, [Wx, self.hc[kc]], [ps])
                    acc = self.get('F')
                    fcw = lambda k, ci=ci: self.P(l, 'fcw', ci * 3 + k, ci * 3 + k + 1)
                    self.act(acc[:, 0:TT], ps[:, :], AF.Identity, [ps, par], [acc], scale=fcw(2), bias=self.P(l, 'fcb', ci, ci + 1))
                    self.cp('act', fhn[:, ci * 2: ci * 2 + 2], ps[:, TT - 2:TT], [ps], [fhn])
                    self.stt(acc[:, 1:TT], ps[:, 0:TT - 1], fcw(1), acc[:, 1:TT], ALU.mult, ALU.add, [ps, par, acc], [acc])
                    self.stt(acc[:, 2:TT], ps[:, 0:TT - 2], fcw(0), acc[:, 2:TT], ALU.mult, ALU.add, [ps, par, acc], [acc])
                    hl = fh[:, ci * 2: ci * 2 + 2]
                    self.stt(acc[:, 0:1], fh[:, ci * 2 + 1: ci * 2 + 2], fcw(1), acc[:, 0:1], ALU.mult, ALU.add, [fh, par, acc], [acc])
                    self.stt(acc[:, 0:2], hl, fcw(0), acc[:, 0:2], ALU.mult, ALU.add, [fh, par, acc], [acc])
                    accs.append(acc)
                pend.append(tuple(accs))
                if len(pend) > 1:
                    self.ffn_fin(pend.pop(0), hid)
        while pend:
            self.ffn_fin(pend.pop(0), hid)
        self.ck(11)
        for m in range(8):
            W, cw = self.wload((l, 'down', m))
            ps = self.nps()
            for kc in range(22):
                self.mm(ps[:, :], W[:, kc * cw: (kc + 1) * cw], hid[kc][:, :], kc == 0, kc == 21, [W, hid[kc]], [ps])
            self.tt(self.xF[:, m * TT:(m + 1) * TT], ps[:, :], self.xF[:, m * TT:(m + 1) * TT], ALU.add, [ps, self.xFc[m]], [self.xFc[m]])
        self.free(*hid)
        self.dump('x2', self.xF, self.xF[:, :])


def _mk_eps(b):
    b.epsT = b.fw.sb([128, 1], F32)
    b.memset('pool', b.epsT, b.epsT[:], EPS)


_orig_alloc = Builder.alloc_all


def _alloc_all(self, nslots):
    _orig_alloc(self, nslots)
    _mk_eps(self)


Builder.alloc_all = _alloc_all


def make_in_maps(inputs, nseq, L, layers, ncores):
    consts = const_tables(L)
    maps = []
    shared = {}
    for l in layers:
        shared["w_in%d" % l] = np.ascontiguousarray(inputs['w_in'][l])
        shared["w_out%d" % l] = np.ascontiguousarray(inputs['w_out'][l])
        shared["w_up%d" % l] = np.ascontiguousarray(inputs['ffn_w_up'][l])
        shared["w_down%d" % l] = np.ascontiguousarray(inputs['ffn_w_down'][l])
        shared["par%d" % l] = pack_params(inputs, l)
    shared["fnw"] = np.ascontiguousarray(inputs['final_norm_w'].reshape(8, 128).T)
    for k, v in consts.items():
        shared[k] = v
    return shared


N_CORES = 8


def kernel(**inputs):
    inputs = {k: np.asarray(v) for k, v in inputs.items()}
    x = inputs['x']
    Bt, L, _ = x.shape
    nseq = Bt // N_CORES
    layers = list(range(inputs['w_in'].shape[0]))
    b = Builder(nseq, L, layers, final=True)
    shared = make_in_maps(inputs, nseq, L, layers, N_CORES)
    in_maps = []
    for c in range(N_CORES):
        m = dict(shared)
        m["x"] = np.ascontiguousarray(x[c * nseq:(c + 1) * nseq])
        in_maps.append(m)
    res = run_bass_kernel_spmd(b.nc, in_maps, core_ids=list(range(N_CORES)))
    out = np.concatenate([np.asarray(r["y"]) for r in res.results], axis=0)
    return out.astype(np.float32)
```

```python
import numpy as np
from contextlib import ExitStack
import concourse.bass as bass
import concourse.mybir as mybir
from concourse.bass_utils import run_bass_kernel_spmd

F32 = mybir.dt.float32
BF16 = mybir.dt.bfloat16
AF = mybir.ActivationFunctionType
ALU = mybir.AluOpType
AX = mybir.AxisListType

D = 1024
DPROJ = 3080
DFF = 2816
EPS = 1e-6
TT = 512
NST = 4
ENG = ['pe', 'act', 'dve', 'pool', 'sp']


class T:
    __slots__ = ('t', 'name', 'w', 'r', 'pool')

    def __init__(self, t, name, pool=None):
        self.t = t
        self.name = name
        self.w = None
        self.r = {}
        self.pool = pool

    def __getitem__(self, k):
        return self.t[k]


class FW:
    def __init__(self, nc, n_dma_sems=32):
        self.nc = nc
        self.es = ExitStack()
        self.eng = {'pe': nc.tensor, 'act': nc.scalar, 'dve': nc.vector, 'pool': nc.gpsimd, 'sp': nc.sync}
        self.sem = {e: self.es.enter_context(nc.semaphore('s_' + e)) for e in ENG}
        self.cnt = {e: 0 for e in ENG}
        self.waited = {e: {} for e in ENG}
        self.prog = {e: [] for e in ENG}
        self.dsem = [self.es.enter_context(nc.semaphore('d%d' % i)) for i in range(n_dma_sems)]
        self.dcnt = [0] * n_dma_sems
        self.dnext = 0
        self.n_hw = n_dma_sems
        self.ntile = 0

    def sb(self, shape, dtype, name=None):
        self.ntile += 1
        name = name or ('t%d' % self.ntile)
        t = self.es.enter_context(self.nc.sbuf_tensor(name, list(shape), dtype))
        return T(t, name)

    def ps(self, shape, dtype, name=None):
        self.ntile += 1
        name = name or ('p%d' % self.ntile)
        t = self.es.enter_context(self.nc.psum_tensor(name, list(shape), dtype))
        return T(t, name)

    def _need(self, e, dep):
        if dep is None:
            return
        if dep[0] == 'e':
            _, f, n = dep
            assert n <= self.cnt[f], "unissued ticket %s %d>%d (engine %s)" % (f, n, self.cnt[f], e)
            key = ('e', f)
            sem = self.sem[f]
        else:
            _, si, n = dep
            key = ('d', si)
            sem = self.dsem[si]
        if self.waited[e].get(key, 0) >= n:
            return
        self.waited[e][key] = n
        eng = self.eng[e]
        self.prog[e].append(lambda eng=eng, sem=sem, n=n: eng.wait_ge(sem, n))

    def op(self, e, fn, reads=(), writes=(), inc=True):
        pe = (e == 'pe')
        for t in reads:
            if t.w is not None and not (pe and t.w[0] == 'e' and t.w[1] == 'pe'):
                self._need(e, t.w)
        for t in writes:
            if t.w is not None and not (pe and t.w[0] == 'e' and t.w[1] == 'pe'):
                self._need(e, t.w)
            for k, d in t.r.items():
                if pe and d[0] == 'e' and d[1] == 'pe':
                    continue
                self._need(e, d)
        if inc:
            self.cnt[e] += 1
            n = self.cnt[e]
            sem = self.sem[e]
            self.prog[e].append(lambda eng=self.eng[e], fn=fn, sem=sem: fn(eng).then_inc(sem, 1))
        else:
            n = self.cnt[e] + 1
            self.prog[e].append(lambda eng=self.eng[e], fn=fn: fn(eng))
        dep = ('e', e, n)
        for t in reads:
            t.r[e] = dep
        for t in writes:
            t.w = dep
            t.r = {}
        return dep

    def dma(self, e, out_ap, in_ap, reads=(), writes=(), **kw):
        for t in reads:
            if t.w is not None:
                self._need(e, t.w)
        for t in writes:
            if t.w is not None:
                self._need(e, t.w)
            for k, d in t.r.items():
                self._need(e, d)
        if e == 'pool':
            self.dsem.append(self.es.enter_context(self.nc.semaphore('q%d' % len(self.dsem))))
            self.dcnt.append(0)
            semi = len(self.dsem) - 1
        else:
            semi = self.dnext
            self.dnext = (self.dnext + 1) % self.n_hw
            if self.dcnt[semi] > 0:
                self._need(e, ('d', semi, self.dcnt[semi]))
        self.dcnt[semi] += 16
        n = self.dcnt[semi]
        sem = self.dsem[semi]
        self.prog[e].append(lambda eng=self.eng[e], o=out_ap, i=in_ap, sem=sem, kw=kw:
                            eng.dma_start(out=o, in_=i, **kw).then_inc(sem, 16))
        dep = ('d', semi, n)
        for t in reads:
            t.r[('d', semi)] = dep
        for t in writes:
            t.w = dep
            t.r = {}
        return dep

    def wait_all_dma(self, e):
        for si in range(len(self.dsem)):
            if self.dcnt[si] > 0:
                self._need(e, ('d', si, self.dcnt[si]))

    def emit(self):
        nc = self.nc
        with nc.Block() as block:
            @block.tensor
            def _(t):
                for f in self.prog['pe']:
                    f()

            @block.scalar
            def _(t):
                for f in self.prog['act']:
                    f()

            @block.vector
            def _(t):
                for f in self.prog['dve']:
                    f()

            @block.gpsimd
            def _(t):
                for f in self.prog['pool']:
                    f()

            @block.sync
            def _(t):
                for f in self.prog['sp']:
                    f()
        self.es.close()


def _pack_layout():
    off = {}
    c = 0
    for name, n in [('n1w', 8), ('n2w', 8), ('scw', 32), ('scb', 8), ('dtb', 8), ('alog', 8), ('dsk', 8),
                    ('snw', 512), ('lcw', 8), ('lcb', 2), ('ba', 2), ('bx', 2), ('lam', 2),
                    ('fcw', 132), ('fcb', 44), ('wa', 256), ('wx', 256)]:
        off[name] = (c, n)
        c += n
    return off, c


POFF, PCOLS = _pack_layout()


def pack_params(inp, l):
    P = np.zeros((128, PCOLS), np.float32)

    def put(name, arr):
        o, n = POFF[name]
        assert arr.shape == (128, n), (name, arr.shape)
        P[:, o:o + n] = arr

    fm = lambda v: np.ascontiguousarray(v.reshape(-1, 128).T)
    bc = lambda v: np.ascontiguousarray(np.broadcast_to(v[None, :], (128, v.shape[0])))
    put('n1w', fm(inp['norm1_w'][l]))
    put('n2w', fm(inp['norm2_w'][l]))
    cw = inp['ssd_conv_w'][l]
    put('scw', np.ascontiguousarray(cw.reshape(4, 8, 128).transpose(2, 1, 0)).reshape(128, 32))
    put('scb', fm(inp['ssd_conv_b'][l]))
    put('dtb', bc(inp['ssd_dt_bias'][l]))
    put('alog', bc(inp['ssd_a_log'][l]))
    put('dsk', bc(inp['ssd_d'][l]))
    put('snw', bc(inp['ssd_norm_w'][l]))
    lw = inp['lru_conv_w'][l]
    put('lcw', np.ascontiguousarray(lw.reshape(4, 2, 128).transpose(2, 1, 0)).reshape(128, 8))
    put('lcb', fm(inp['lru_conv_b'][l]))
    put('ba', fm(inp['lru_b_a'][l]))
    put('bx', fm(inp['lru_b_x'][l]))
    put('lam', fm(inp['lru_lambda'][l]))
    fw_ = inp['ffn_conv_w'][l]
    put('fcw', np.ascontiguousarray(fw_.reshape(3, 44, 128).transpose(2, 1, 0)).reshape(128, 132))
    put('fcb', fm(inp['ffn_conv_b'][l]))
    for nm, key in (('wa', 'lru_w_a'), ('wx', 'lru_w_x')):
        w = inp[key][l]
        bd = np.zeros((128, 2, 128), np.float32)
        for k in range(4):
            c_, hh = k // 2, k % 2
            bd[hh * 64:(hh + 1) * 64, c_, hh * 64:(hh + 1) * 64] = w[k]
        put(nm, bd.reshape(128, 256))
    return P


def const_tables(L):
    nsub = L // 128
    half = 32
    inv = (10000.0 ** (-np.arange(half, dtype=np.float32) / np.float32(half))).astype(np.float32)
    pos = (np.arange(nsub)[None, :] * 128 + np.arange(128)[:, None]).astype(np.float32)
    ang = (pos[:, :, None] * inv[None, None, :]).astype(np.float32)
    cos = np.cos(ang).astype(np.float32).reshape(128, nsub * 32)
    sin = np.sin(ang).astype(np.float32).reshape(128, nsub * 32)
    lg = np.log(1.0 - 2.0 ** (-5.0 - np.arange(4, dtype=np.float64)))
    s = np.arange(128)[:, None]
    l_ = np.arange(128)[None, :]
    diff = l_ - s
    rdm = np.zeros((128, 4, 128), np.float64)
    for h in range(4):
        rdm[:, h, :] = np.where(diff >= 0, np.exp(lg[h] * np.maximum(diff, 0)), 0.0) * 0.125
    rqd = np.zeros((128, 2, 128), np.float64)
    rcd = np.zeros((128, 2), np.float64)
    for c in range(2):
        for hh in range(2):
            h = 2 * c + hh
            rqd[hh * 64:(hh + 1) * 64, c, :] = 0.125 * np.exp(lg[h] * (np.arange(128) + 1.0))[None, :]
            rcd[hh * 64:(hh + 1) * 64, c] = np.exp(lg[h] * 128.0)
    rkd = np.exp(lg[None, :] * (127.0 - np.arange(128))[:, None])
    return dict(cos=cos, sin=sin, rdm=rdm.reshape(128, 512).astype(np.float32),
                rqd=rqd.reshape(128, 256).astype(np.float32), rcd=rcd.astype(np.float32),
                rkd=rkd.astype(np.float32))


IN_SLABS = [(0, 512), (512, 512), (2560, 520), (1024, 512), (1536, 512), (2048, 512)]
UP_SLABS = []
for _s in range(6):
    _w = 512 if _s < 5 else 256
    UP_SLABS.append((_s * 512, _w))
    UP_SLABS.append((DFF + _s * 512, _w))
SLOTW = 4160


class StopBuild(Exception):
    pass


class Builder:
    stop = 99

    def ck(self, n):
        if self.stop == n:
            raise StopBuild()

    def __init__(self, nseq, L, layers, depth_total=2, final=True, dbg=None, nslots=4):
        self.nseq, self.L, self.layers, self.final = nseq, L, list(layers), final
        self.ntile = L // TT
        self.nsub = L // 128
        self.dbg = dbg or []
        nc = self.nc = bass.Bass("TRN2", target_bir_lowering=False)
        fw = self.fw = FW(nc)
        dt_in = lambda name, shape: nc.dram_tensor(name, list(shape), F32, kind="ExternalInput").ap()
        self.x = dt_in("x", [nseq, L, D])
        self.y = nc.dram_tensor("y", [nseq, L, D], F32, kind="ExternalOutput").ap()
        self.wd = {}
        for l in self.layers:
            self.wd[(l, 'in')] = dt_in("w_in%d" % l, [D, DPROJ])
            self.wd[(l, 'out')] = dt_in("w_out%d" % l, [D, D])
            self.wd[(l, 'up')] = dt_in("w_up%d" % l, [D, 2 * DFF])
            self.wd[(l, 'down')] = dt_in("w_down%d" % l, [DFF, D])
            self.wd[(l, 'par')] = dt_in("par%d" % l, [128, PCOLS])
        self.fnw_d = dt_in("fnw", [128, 8])
        self.cos_d = dt_in("cos", [128, self.nsub * 32])
        self.sin_d = dt_in("sin", [128, self.nsub * 32])
        self.rdm_d = dt_in("rdm", [128, 512])
        self.rqd_d = dt_in("rqd", [128, 256])
        self.rcd_d = dt_in("rcd", [128, 2])
        self.rkd_d = dt_in("rkd", [128, 4])
        self.dbg_out = {}
        for name, shape in self.dbg:
            self.dbg_out[name] = nc.dram_tensor("dbg_" + name, list(shape), F32, kind="ExternalOutput").ap()
        self.scr = {}
        for l in self.layers:
            for mat, slabs, kcn in (('in', IN_SLABS, 8), ('out', [(0, 512), (512, 512)], 8), ('up', UP_SLABS, 8),
                                    ('down', [(m * 128, 128) for m in range(8)], 22)):
                for s, (c0, cw) in enumerate(slabs):
                    t = nc.dram_tensor("scr_%d_%s_%d" % (l, mat, s), [128, kcn, cw], BF16).ap()
                    self.scr[(l, mat, s)] = (T(t, 'scr'), c0, cw, kcn)
        self.alloc_all(nslots)
        self.build()
        fw.emit()

    def alloc_all(self, nslots):
        fw = self.fw
        sb = fw.sb
        self.ident_f = sb([128, 128], F32)
        self.ident_b = sb([128, 128], BF16)
        self.U = sb([128, 128], F32)
        self.ones_f = sb([128, 128], F32)
        self.ones_b = sb([128, 128], BF16)
        self.cosT = sb([128, self.nsub * 32], F32)
        self.sinT = sb([128, self.nsub * 32], F32)
        self.rdm = sb([128, 512], F32)
        self.rqd = sb([128, 256], F32)
        self.rcd = sb([128, 2], F32)
        self.rkd = sb([128, 4], F32)
        self.fnw = sb([128, 8], F32)
        self.par = {l: sb([128, PCOLS], F32) for l in self.layers}
        self.aneg = {l: sb([128, 8], F32) for l in self.layers}
        self.DI = {l: sb([128, 8 * 128], BF16) for l in self.layers}
        self.cneg = {l: sb([128, 4], F32) for l in self.layers}
        self.wab = {l: sb([128, 256], BF16) for l in self.layers}
        self.wxb = {l: sb([128, 256], BF16) for l in self.layers}
        self.R = {l: sb([128, 128], F32) for l in self.layers}
        self.Rb = {l: sb([128, 128], BF16) for l in self.layers}
        self.S = {l: sb([128, 512], F32) for l in self.layers}
        self.Sb = {l: sb([128, 512], BF16) for l in self.layers}
        self.hst = {l: sb([128, 2], F32) for l in self.layers}
        self.shalo = {l: sb([128, 8 * 3], F32) for l in self.layers}
        self.lhalo = {l: sb([128, 2 * 3], F32) for l in self.layers}
        self.fhalo = {l: [sb([128, 88 * 2], F32) for _ in range(2)] for l in self.layers}
        self.fhc = {l: [[T(self.fhalo[l][p].t, 'fh') for _ in range(88)] for p in range(2)] for l in self.layers}
        self.xin = [sb([128, D], F32) for _ in range(2)]
        self.xin_i = 0
        self.ost = [sb([128, D], F32) for _ in range(2)]
        self.ost_i = 0
        self.xF = sb([128, 8 * TT], F32)
        self.xFc = [T(self.xF.t, 'xF%d' % m) for m in range(8)]
        self.h = sb([128, 8 * TT], BF16)
        self.yF = sb([128, 8 * TT], BF16)
        self.hc = [T(self.h.t, 'h%d' % m) for m in range(8)]
        self.yFc = [T(self.yF.t, 'yF%d' % m) for m in range(8)]
        self.qkT = sb([128, 4 * TT], BF16)
        self.qz = sb([128, 4 * TT], BF16)
        self.qdz = sb([128, 4 * TT], BF16)
        self.wslot = [sb([128, SLOTW], BF16) for _ in range(nslots)]
        self.wslot_i = 0
        self.poolF = [T(sb([128, 515], F32).t, 'F%d' % i, 'F') for i in range(14)]
        self.poolH = [T(sb([128, 512], BF16).t, 'H%d' % i, 'H') for i in range(34)]
        self.poolS = [T(sb([128, 32], F32).t, 'S%d' % i, 'S') for i in range(20)]
        self.poolM = [T(sb([128, 128], BF16).t, 'M%d' % i, 'M') for i in range(16)]
        self.poolL = [T(sb([128, 128], F32).t, 'L%d' % i, 'L') for i in range(10)]
        self.pools = {'F': self.poolF, 'H': self.poolH, 'S': self.poolS, 'M': self.poolM, 'L': self.poolL}
        self.psb = [fw.ps([128, 512], F32) for _ in range(8)]
        self.ps_i = 0

    def get(self, p):
        lst = self.pools[p]
        assert lst, "pool %s empty" % p
        return lst.pop(0)

    def free(self, *ts):
        for t in ts:
            self.pools[t.pool].append(t)

    def nps(self):
        t = self.psb[self.ps_i]
        self.ps_i = (self.ps_i + 1) % 8
        return t

    def mm(self, out_ap, lhsT, rhs, start, stop, reads, writes, inc=None):
        if inc is None:
            inc = stop
        self.fw.op('pe', lambda t: t.matmul(out_ap, lhsT, rhs, start=start, stop=stop), reads, writes, inc=inc)

    def tr(self, out_ap, in_ap, ident_ap, reads, writes, inc=True):
        self.fw.op('pe', lambda t: t.transpose(out_ap, in_ap, ident_ap), reads, writes, inc=inc)

    def act(self, out_ap, in_ap, func, reads, writes, **kw):
        self.fw.op('act', lambda a: a.activation(out=out_ap, in_=in_ap, func=func, **kw), reads, writes)

    def tt(self, out_ap, in0, in1, op, reads, writes, eng='dve'):
        self.fw.op(eng, lambda v: v.tensor_tensor(out=out_ap, in0=in0, in1=in1, op=op), reads, writes)

    def ts(self, out_ap, in0, s1, s2, op0, op1, reads, writes, eng='dve'):
        if op1 is None:
            self.fw.op(eng, lambda v: v.tensor_scalar(out=out_ap, in0=in0, scalar1=s1, scalar2=None, op0=op0), reads, writes)
        else:
            self.fw.op(eng, lambda v: v.tensor_scalar(out=out_ap, in0=in0, scalar1=s1, scalar2=s2, op0=op0, op1=op1), reads, writes)

    def stt(self, out_ap, in0, scalar, in1, op0, op1, reads, writes):
        self.fw.op('dve', lambda v: v.scalar_tensor_tensor(out=out_ap, in0=in0, scalar=scalar, in1=in1, op0=op0, op1=op1), reads, writes)

    def cp(self, eng, out_ap, in_ap, reads, writes):
        if eng == 'act':
            self.fw.op('act', lambda a: a.copy(out=out_ap, in_=in_ap), reads, writes)
        else:
            self.fw.op(eng, lambda v: v.tensor_copy(out=out_ap, in_=in_ap), reads, writes)

    def memset(self, eng, t, ap, val):
        self.fw.op(eng, lambda v: v.memset(ap, val), [], [t])

    def P(self, l, name, a=None, b=None):
        o, n = POFF[name]
        if a is None:
            return self.par[l][:, o:o + n]
        return self.par[l][:, o + a:o + b]

    def dump(self, name, t, ap):
        if name in self.dbg_out:
            self.fw.dma('pool', self.dbg_out[name], ap, reads=[t])

    def build_wsched(self):
        sched = []
        for seq in range(self.nseq):
            for ti in range(self.ntile):
                for l in self.layers:
                    for s in range(6):
                        sched.append((l, 'in', s))
                    for s in range(2):
                        sched.append((l, 'out', s))
                    for s in range(12):
                        sched.append((l, 'up', s))
                    for s in range(8):
                        sched.append((l, 'down', s))
        self.wsched = sched
        self.w_loaded = 0
        self.w_used = 0
        self.w_slotof = {}

    def wload(self, key):
        assert self.wsched[self.w_used] == key, (self.wsched[self.w_used], key)
        ns = len(self.wslot)
        while self.w_loaded < len(self.wsched) and self.w_loaded < self.w_used + ns - 1:
            k = self.wsched[self.w_loaded]
            st, c0, cw, kcn = self.scr[k]
            slot = self.wslot[self.w_loaded % ns]
            self.fw.dma('sp', slot[:, 0:kcn * cw], st.t.rearrange("p k c -> p (k c)"), reads=[st], writes=[slot])
            self.w_loaded += 1
        slot = self.wslot[self.w_used % ns]
        self.w_used += 1
        return slot, self.scr[key][2]

    def build(self):
        fw = self.fw
        self.memset('pool', self.ones_f, self.ones_f[:], 1.0)
        self.memset('pool', self.qz, self.qz[:], 0.0)
        self.memset('pool', self.qdz, self.qdz[:], 0.0)
        self.memset('pool', self.ones_b, self.ones_b[:], 1.0)
        self.memset('pool', self.U, self.U[:], 1.0)
        fw.op('pool', lambda g: g.affine_select(out=self.U[:], in_=self.U[:], pattern=[[1, 128]], compare_op=ALU.is_ge,
                                                fill=0.0, base=0, channel_multiplier=-1), [self.U], [self.U])
        self.memset('pool', self.ident_f, self.ident_f[:], 1.0)
        fw.op('pool', lambda g: g.affine_select(out=self.ident_f[:], in_=self.ident_f[:], pattern=[[1, 128]], compare_op=ALU.is_equal,
                                                fill=0.0, base=0, channel_multiplier=-1), [self.ident_f], [self.ident_f])
        self.cp('dve', self.ident_b[:], self.ident_f[:], [self.ident_f], [self.ident_b])
        for t, d in ((self.cosT, self.cos_d), (self.sinT, self.sin_d), (self.rdm, self.rdm_d), (self.rqd, self.rqd_d),
                     (self.rcd, self.rcd_d), (self.rkd, self.rkd_d), (self.fnw, self.fnw_d)):
            fw.dma('sp', t[:], d, writes=[t])
        for l in self.layers:
            fw.dma('sp', self.par[l][:], self.wd[(l, 'par')], writes=[self.par[l]])
        for l in self.layers:
            for mat, n in (('in', 6), ('out', 2), ('up', 12), ('down', 8)):
                for s in range(n):
                    st, c0, cw, kcn = self.scr[(l, mat, s)]
                    src = self.wd[(l, mat)][:, c0:c0 + cw].rearrange("(k p) c -> p k c", p=128)
                    fw.dma('pool', st.t, src, writes=[st])
        for l in self.layers:
            par = self.par[l]
            self.act(self.aneg[l][:], self.P(l, 'alog'), AF.Exp, [par], [self.aneg[l]])
            self.ts(self.aneg[l][:], self.aneg[l][:], -1.0, None, ALU.mult, None, [self.aneg[l]], [self.aneg[l]])
            for hd in range(8):
                self.ts(self.DI[l][:, hd * 128:(hd + 1) * 128], self.ident_f[:], self.P(l, 'dsk', hd, hd + 1), None, ALU.mult, None,
                        [self.ident_f, par], [self.DI[l]])
            tmp = self.get('S')
            self.act(tmp[:, 0:2], self.P(l, 'lam'), AF.Exp, [par], [tmp], scale=-1.0)
            self.act(tmp[:, 0:2], tmp[:, 0:2], AF.Ln, [tmp], [tmp], bias=1.0)
            self.ts(self.cneg[l][:, 0:2], tmp[:, 0:2], -8.0, None, ALU.mult, None, [tmp], [self.cneg[l]])
            self.ts(self.cneg[l][:, 2:4], tmp[:, 0:2], -16.0, None, ALU.mult, None, [tmp], [self.cneg[l]])
            self.free(tmp)
            self.cp('dve', self.wab[l][:], self.P(l, 'wa'), [par], [self.wab[l]])
            self.cp('dve', self.wxb[l][:], self.P(l, 'wx'), [par], [self.wxb[l]])
        self.build_wsched()
        for seq in range(self.nseq):
            for l in self.layers:
                for t in (self.R[l], self.Rb[l], self.S[l], self.Sb[l], self.hst[l], self.shalo[l], self.lhalo[l]):
                    self.memset('pool', t, t[:], 0.0)
                for p in range(2):
                    fht = self.fhalo[l][p]
                    self.fw.op('pool', lambda v, fht=fht: v.memset(fht[:], 0.0), [], self.fhc[l][p])
            for ti in range(self.ntile):
                self.load_x(seq, ti)
                try:
                    for l in self.layers:
                        self.layer(l, ti)
                except StopBuild:
                    pass
                self.store_out(seq, ti)
        fw.wait_all_dma('sp')

    def load_x(self, seq, ti):
        fw = self.fw
        for st in range(NST):
            xin = self.xin[self.xin_i]
            self.xin_i ^= 1
            t0 = ti * TT + st * 128
            fw.dma('sp', xin[:], self.x[seq, t0:t0 + 128, :], writes=[xin])
            for half in range(2):
                ps = self.nps()
                for k4 in range(4):
                    kc = half * 4 + k4
                    self.tr(ps[:, k4 * 128:(k4 + 1) * 128], xin[:, kc * 128:(kc + 1) * 128], self.ident_f[:], [xin, self.ident_f], [ps], inc=(k4 == 3))
                dst = self.xF[:, half * 4 * TT:(half + 1) * 4 * TT].rearrange("p (k t) -> p k t", k=4)[:, :, st * 128:(st + 1) * 128]
                self.cp('act', dst, ps[:, :].rearrange("p (k t) -> p k t", k=4), [ps], self.xFc[half * 4:(half + 1) * 4])

    def rms_rstd(self, src):
        sq = [self.get('H') for _ in range(8)]
        for kc in range(8):
            self.act(sq[kc][:, :], src[:, kc * TT:(kc + 1) * TT], AF.Square, [self.xFc[kc]], [sq[kc]])
        ps = self.nps()
        for kc in range(8):
            self.mm(ps[:, :], self.ones_b[:], sq[kc][:, :], kc == 0, kc == 7, [self.ones_b, sq[kc]], [ps])
        self.free(*sq)
        r = self.get('F')
        self.act(r[:, 0:TT], ps[:, :], AF.Ln, [ps], [r], scale=1.0 / D, bias=self.epsT[:, 0:1])
        self.act(r[:, 0:TT], r[:, 0:TT], AF.Exp, [r], [r], scale=-0.5)
        return r

    def norm_to_h(self, l, wname):
        r = self.rms_rstd(self.xF)
        for kc in range(8):
            self.stt(self.h[:, kc * TT:(kc + 1) * TT], self.xF[:, kc * TT:(kc + 1) * TT], self.P(l, wname, kc, kc + 1), r[:, 0:TT],
                     ALU.mult, ALU.mult, [self.xFc[kc], self.par[l], r], [self.hc[kc]])
        self.free(r)

    def store_out(self, seq, ti):
        fw = self.fw
        if self.final:
            r = self.rms_rstd(self.xF)
            yo = [self.get('F') for _ in range(8)]
            for kc in range(8):
                self.stt(yo[kc][:, 0:TT], self.xF[:, kc * TT:(kc + 1) * TT], self.fnw[:, kc:kc + 1], r[:, 0:TT], ALU.mult, ALU.mult,
                         [self.xFc[kc], self.fnw, r], [yo[kc]])
            self.free(r)
            srcs = yo
            get = lambda kc, st: yo[kc][:, st * 128:(st + 1) * 128]
        else:
            srcs = list(self.xFc)
            get = lambda kc, st: self.xF[:, kc * TT + st * 128: kc * TT + (st + 1) * 128]
        for st in range(NST):
            ost = self.ost[self.ost_i]
            self.ost_i ^= 1
            for half in range(2):
                ps = self.nps()
                for k4 in range(4):
                    kc = half * 4 + k4
                    self.tr(ps[:, k4 * 128:(k4 + 1) * 128], get(kc, st), self.ident_f[:], [srcs[kc], self.ident_f], [ps], inc=(k4 == 3))
                self.cp('act', ost[:, half * 512:(half + 1) * 512], ps[:, :], [ps], [ost])
            t0 = ti * TT + st * 128
            fw.dma('act', self.y[seq, t0:t0 + 128, :], ost[:], reads=[ost])
        if self.final:
            self.free(*yo)

    def ffn_fin(self, accs, hid):
        au, av = accs
        self.act(au[:, 0:TT], au[:, 0:TT], AF.Gelu_apprx_tanh, [au], [au])
        o = self.get('H')
        self.tt(o[:, :], au[:, 0:TT], av[:, 0:TT], ALU.mult, [au, av], [o], eng='pool')
        self.free(au, av)
        hid.append(o)

    def layer(self, l, ti):
        par = self.par[l]
        h = self.h
        hk = lambda kc, a=0, b=TT: h[:, kc * TT + a: kc * TT + b]
        sub0 = ti * NST
        self.ck(0)
        self.norm_to_h(l, 'n1w')
        self.ck(1)
        W, cw = self.wload((l, 'in', 0))
        qk_TM = [self.get('H') for _ in range(NST)]
        for st in range(NST):
            ps = self.nps()
            for kc in range(8):
                self.mm(ps[:, :], hk(kc, st * 128, (st + 1) * 128), W[:, kc * cw: kc * cw + 512], kc == 0, kc == 7, [self.hc[kc], W], [ps])
            self.ck(101)
            v4 = ps[:, :].rearrange("p (h t d) -> p h t d", h=8, t=2)
            x1, x2 = v4[:, :, 0, :], v4[:, :, 1, :]
            o4 = qk_TM[st][:, :].rearrange("p (h t d) -> p h t d", h=8, t=2)
            sub = sub0 + st
            cosb = self.cosT[:, sub * 32:(sub + 1) * 32].unsqueeze(1).to_broadcast([128, 8, 32])
            sinb = self.sinT[:, sub * 32:(sub + 1) * 32].unsqueeze(1).to_broadcast([128, 8, 32])
            t1, t2 = self.get('F'), self.get('F')
            a1 = t1[:, 0:256].rearrange("p (h d) -> p h d", h=8)
            a2 = t2[:, 0:256].rearrange("p (h d) -> p h d", h=8)
            self.tt(a1, x1, cosb, ALU.mult, [ps, self.cosT], [t1])
            self.tt(a2, x2, sinb, ALU.mult, [ps, self.sinT], [t2])
            self.tt(o4[:, :, 0, :], a1, a2, ALU.subtract, [t1, t2], [qk_TM[st]])
            self.tt(a1, x1, sinb, ALU.mult, [ps, self.sinT], [t1])
            self.tt(a2, x2, cosb, ALU.mult, [ps, self.cosT], [t2])
            self.tt(o4[:, :, 1, :], a1, a2, ALU.add, [t1, t2], [qk_TM[st]])
            self.free(t1, t2)
            self.ck(102)
            pt = self.nps()
            ptb = pt[:, :].bitcast(BF16)
            for c in range(4):
                self.tr(ptb[:, c * 128:(c + 1) * 128], qk_TM[st][:, c * 128:(c + 1) * 128], self.ident_b[:], [qk_TM[st], self.ident_b], [pt], inc=(c == 3))
            self.ck(103)
            dst = self.qkT[:, 2 * TT:4 * TT].rearrange("p (c t) -> p c t", c=2)[:, :, st * 128:(st + 1) * 128]
            self.cp('act', dst, ptb[:, 256:512].rearrange("p (c t) -> p c t", c=2), [pt], [self.qkT])
            self.ck(104)
            for hh in range(2):
                pr = slice(hh * 64, (hh + 1) * 64)
                dz = self.qz[pr, :].rearrange("p (c h t) -> p c h t", c=2, h=2)[:, :, hh, st * 128:(st + 1) * 128]
                ddz = self.qdz[pr, :].rearrange("p (c h t) -> p c h t", c=2, h=2)[:, :, hh, st * 128:(st + 1) * 128]
                self.cp('act', dz, ptb[pr, 0:256].rearrange("p (c t) -> p c t", c=2), [pt], [self.qz])
                self.tt(ddz, dz, self.rqd[pr, :].rearrange("p (c t) -> p c t", c=2), ALU.mult, [self.qz, self.rqd], [self.qdz])
        self.ck(2)
        W, cw = self.wload((l, 'in', 1))
        v_TM = [self.get('H') for _ in range(NST)]
        sg = [self.get('F') for _ in range(NST)]
        for st in range(NST):
            ps = self.nps()
            for kc in range(8):
                self.mm(ps[:, :], hk(kc, st * 128, (st + 1) * 128), W[:, kc * cw: kc * cw + 512], kc == 0, kc == 7, [self.hc[kc], W], [ps])
            self.cp('act', v_TM[st][:, 0:256], ps[:, 0:256], [ps], [v_TM[st]])
            self.tt(v_TM[st][:, 256:512].rearrange("p (h d) -> p h d", h=4), ps[:, 0:256].rearrange("p (h d) -> p h d", h=4),
                    self.rkd[:, 0:4].unsqueeze(2).to_broadcast([128, 4, 64]), ALU.mult, [ps, self.rkd], [v_TM[st]])
            self.act(sg[st][:, 0:256], ps[:, 256:512], AF.Silu, [ps], [sg[st]])
        self.ck(3)
        R, Rb = self.R[l], self.Rb[l]
        for st in range(NST):
            sc = slice(st * 128, (st + 1) * 128)
            ps = self.nps()
            for hd in range(4):
                c, hh = hd // 2, hd % 2
                pr = slice(hh * 64, (hh + 1) * 64)
                self.mm(ps[:, hd * 128:(hd + 1) * 128], self.qkT[:, (2 + c) * TT + st * 128:(2 + c) * TT + (st + 1) * 128],
                        self.qz[:, hd * TT + st * 128: hd * TT + (st + 1) * 128], True, True, [self.qkT, self.qz], [ps], inc=(hd == 3))
            self.ck(201)
            PT = self.get('H')
            self.tt(PT[:, :], ps[:, :], self.rdm[:, :], ALU.mult, [ps, self.rdm], [PT])
            self.ck(202)
            py = self.nps()
            for hd in range(4):
                c, hh = hd // 2, hd % 2
                pr = slice(hh * 64, (hh + 1) * 64)
                self.mm(py[:, hd * 64:(hd + 1) * 64], PT[:, hd * 128:(hd + 1) * 128], v_TM[st][:, hd * 64:(hd + 1) * 64], True, False,
                        [PT, v_TM[st]], [py], inc=False)
                self.mm(py[:, hd * 64:(hd + 1) * 64], self.qdz[:, hd * TT + st * 128: hd * TT + (st + 1) * 128], Rb[:, c * 64:(c + 1) * 64],
                        False, True, [self.qdz, Rb], [py], inc=(hd == 3))
            self.free(PT)
            self.ck(203)
            pk = self.nps()
            for c in range(2):
                self.mm(pk[:, c * 128:(c + 1) * 128], qk_TM[st][:, 256 + c * 128: 256 + (c + 1) * 128], v_TM[st][:, 256 + c * 128: 256 + (c + 1) * 128],
                        True, True, [qk_TM[st], v_TM[st]], [pk], inc=(c == 1))
            self.ck(204)
            for c in range(2):
                for hh in range(2):
                    pr = slice(hh * 64, (hh + 1) * 64)
                    self.stt(R[pr, c * 64:(c + 1) * 64], R[pr, c * 64:(c + 1) * 64], self.rcd[pr, c:c + 1],
                             pk[pr, c * 128 + hh * 64: c * 128 + (hh + 1) * 64], ALU.mult, ALU.add, [R, self.rcd, pk], [R])
            self.ck(205)
            self.cp('act', Rb[:, :], R[:, :], [R], [Rb])
            self.ck(206)
            sm = self.get('S')
            y3 = py[:, 0:256].rearrange("p (h d) -> p h d", h=4)
            self.fw.op('dve', lambda v, sm=sm, y3=y3: v.tensor_reduce(out=sm[:, 0:4], in_=y3, axis=AX.X, op=ALU.add), [py], [sm])
            self.ck(207)
            ysq = self.get('F')
            self.act(ysq[:, 0:256], py[:, 0:256], AF.Square, [py], [ysq])
            self.fw.op('dve', lambda v, sm=sm, ysq=ysq: v.tensor_reduce(out=sm[:, 4:8], in_=ysq[:, 0:256].rearrange("p (h d) -> p h d", h=4),
                                                                      axis=AX.X, op=ALU.add), [ysq], [sm])
            self.ts(sm[:, 0:4], sm[:, 0:4], 1.0 / 64, None, ALU.mult, None, [sm], [sm])
            self.tt(sm[:, 8:12], sm[:, 0:4], sm[:, 0:4], ALU.mult, [sm], [sm])
            self.stt(sm[:, 4:8], sm[:, 4:8], 1.0 / 64, sm[:, 8:12], ALU.mult, ALU.subtract, [sm], [sm])
            self.act(sm[:, 4:8], sm[:, 4:8], AF.Ln, [sm], [sm], bias=self.epsT[:, 0:1])
            self.act(sm[:, 4:8], sm[:, 4:8], AF.Exp, [sm], [sm], scale=-0.5)
            self.ck(208)
            yc = ysq
            yc3 = yc[:, 0:256].rearrange("p (h d) -> p h d", h=4)
            self.tt(yc3, y3, sm[:, 0:4].unsqueeze(2).to_broadcast([128, 4, 64]), ALU.subtract, [py, sm], [yc])
            self.tt(yc3, yc3, sm[:, 4:8].unsqueeze(2).to_broadcast([128, 4, 64]), ALU.mult, [yc, sm], [yc])
            self.ck(209)
            yb = self.get('H')
            self.tt(yb[:, 0:256], yc[:, 0:256], sg[st][:, 0:256], ALU.mult, [yc, sg[st]], [yb])
            self.free(sm, ysq)
            pt = self.nps()
            ptb = pt[:, :].bitcast(BF16)
            for c in range(2):
                self.tr(ptb[:, c * 128:(c + 1) * 128], yb[:, c * 128:(c + 1) * 128], self.ident_b[:], [yb, self.ident_b], [pt], inc=(c == 1))
            self.free(yb)
            dst = self.yF[:, 0:2 * TT].rearrange("p (c t) -> p c t", c=2)[:, :, sc]
            self.cp('act', dst, ptb[:, 0:256].rearrange("p (c t) -> p c t", c=2), [pt], self.yFc[0:2])
        self.free(*qk_TM)
        self.free(*v_TM)
        self.free(*sg)
        self.ck(4)
        W, cw = self.wload((l, 'in', 2))
        pdt = self.nps()
        for st in range(NST):
            for kc in range(8):
                self.mm(pdt[:, st * 8:(st + 1) * 8], hk(kc, st * 128, (st + 1) * 128), W[:, kc * cw: kc * cw + 8], kc == 0, kc == 7,
                        [self.hc[kc], W], [pdt], inc=(kc == 7 and st == NST - 1))
        Sx, Sa, Sdt, SdA, Sln, Scum, Stot, Sncl, Secum, Sdec, Swg = [self.get('S') for _ in range(11)]
        b3 = lambda ap: ap.unsqueeze(1).to_broadcast([128, 4, 8])
        v3 = lambda t: t[:, 0:32].rearrange("p (s h) -> p s h", s=4)
        self.tt(v3(Sx), pdt[:, 0:32].rearrange("p (s h) -> p s h", s=4), b3(self.P(l, 'dtb')), ALU.add, [pdt, par], [Sx])
        self.act(Sa[:, 0:32], Sx[:, 0:32], AF.Abs, [Sx], [Sa])
        self.act(Sa[:, 0:32], Sa[:, 0:32], AF.Exp, [Sa], [Sa], scale=-1.0)
        self.act(Sa[:, 0:32], Sa[:, 0:32], AF.Ln, [Sa], [Sa], bias=1.0)
        self.stt(Sdt[:, 0:32], Sx[:, 0:32], 0.0, Sa[:, 0:32], ALU.max, ALU.add, [Sx, Sa], [Sdt])
        self.tt(v3(SdA), v3(Sdt), b3(self.aneg[l][:, 0:8]), ALU.mult, [Sdt, self.aneg[l]], [SdA])
        self.act(Sln[:, 0:32], Sdt[:, 0:32], AF.Ln, [Sdt], [Sln])
        pc = self.nps()
        self.mm(pc[:, 0:32], self.U[:], SdA[:, 0:32], True, True, [self.U, SdA], [pc], inc=False)
        self.mm(pc[:, 32:64], self.ones_f[:], SdA[:, 0:32], True, True, [self.ones_f, SdA], [pc])
        self.cp('act', Scum[:, 0:32], pc[:, 0:32], [pc], [Scum])
        self.cp('act', Stot[:, 0:32], pc[:, 32:64], [pc], [Stot])
        self.act(Secum[:, 0:32], Scum[:, 0:32], AF.Exp, [Scum], [Secum])
        self.act(Sdec[:, 0:32], Stot[:, 0:32], AF.Exp, [Stot], [Sdec])
        self.tt(Swg[:, 0:32], Stot[:, 0:32], Scum[:, 0:32], ALU.subtract, [Stot, Scum], [Swg])
        self.act(Swg[:, 0:32], Swg[:, 0:32], AF.Exp, [Swg], [Swg])
        self.tt(Swg[:, 0:32], Swg[:, 0:32], Sdt[:, 0:32], ALU.mult, [Swg, Sdt], [Swg])
        self.free(Sx, Sa, Stot, Sncl)
        self.ck(5)
        gg = []
        for c in range(2):
            ps = self.nps()
            for kc in range(8):
                self.mm(ps[:, :], W[:, kc * cw + 8 + c * 128: kc * cw + 8 + (c + 1) * 128], hk(kc), kc == 0, kc == 7, [W, self.hc[kc]], [ps])
            g_ = self.get('F')
            self.act(g_[:, 0:TT], ps[:, :], AF.Gelu_apprx_tanh, [ps], [g_])
            gg.append(g_)
        for c in range(2):
            ps = self.nps()
            for kc in range(8):
                self.mm(ps[:, :], W[:, kc * cw + 264 + c * 128: kc * cw + 264 + (c + 1) * 128], hk(kc), kc == 0, kc == 7, [W, self.hc[kc]], [ps])
            raw = self.get('F')
            halo = self.lhalo[l]
            self.cp('pool', raw[:, 0:3], halo[:, c * 3:(c + 1) * 3], [halo], [raw])
            self.cp('act', raw[:, 3:515], ps[:, :], [ps], [raw])
            xc = self.get('F')
            lcw = lambda k: self.P(l, 'lcw', c * 4 + k, c * 4 + k + 1)
            self.act(xc[:, 0:TT], raw[:, 3:515], AF.Identity, [raw, par], [xc], scale=lcw(3), bias=self.P(l, 'lcb', c, c + 1))
            for k in (2, 1, 0):
                self.stt(xc[:, 0:TT], raw[:, k:k + TT], lcw(k), xc[:, 0:TT], ALU.mult, ALU.add, [raw, par, xc], [xc])
            self.cp('pool', halo[:, c * 3:(c + 1) * 3], raw[:, 512:515], [raw], [halo])
            self.free(raw)
            xcb = self.get('H')
            self.cp('act', xcb[:, :], xc[:, 0:TT], [xc], [xcb])
            pr_ = self.nps()
            self.mm(pr_[:, :], self.wab[l][:, c * 128:(c + 1) * 128], xcb[:, :], True, True, [self.wab[l], xcb], [pr_])
            pi_ = self.nps()
            self.mm(pi_[:, :], self.wxb[l][:, c * 128:(c + 1) * 128], xcb[:, :], True, True, [self.wxb[l], xcb], [pi_])
            self.free(xcb)
            r_, i_, a_ = self.get('F'), self.get('F'), self.get('F')
            self.act(r_[:, 0:TT], pr_[:, :], AF.Sigmoid, [pr_, par], [r_], bias=self.P(l, 'ba', c, c + 1))
            self.act(i_[:, 0:TT], pi_[:, :], AF.Sigmoid, [pi_, par], [i_], bias=self.P(l, 'bx', c, c + 1))
            self.act(a_[:, 0:TT], r_[:, 0:TT], AF.Exp, [r_, self.cneg[l]], [a_], scale=self.cneg[l][:, c:c + 1])
            self.act(r_[:, 0:TT], r_[:, 0:TT], AF.Exp, [r_, self.cneg[l]], [r_], scale=self.cneg[l][:, 2 + c:3 + c])
            self.act(r_[:, 0:TT], r_[:, 0:TT], AF.Sqrt, [r_], [r_], scale=-1.0, bias=1.0)
            self.tt(i_[:, 0:TT], i_[:, 0:TT], xc[:, 0:TT], ALU.mult, [i_, xc], [i_])
            self.tt(i_[:, 0:TT], i_[:, 0:TT], r_[:, 0:TT], ALU.mult, [i_, r_], [i_])
            hs = self.hst[l]
            self.fw.op('dve', lambda v, xc=xc, a_=a_, i_=i_, hs=hs, c=c: v.tensor_tensor_scan(
                out=xc[:, 0:TT], data0=a_[:, 0:TT], data1=i_[:, 0:TT], initial=hs[:, c:c + 1], op0=ALU.mult, op1=ALU.add),
                [a_, i_, hs], [xc])
            self.cp('dve', hs[:, c:c + 1], xc[:, TT - 1:TT], [xc], [hs])
            self.tt(self.yF[:, (6 + c) * TT:(7 + c) * TT], xc[:, 0:TT], gg[c][:, 0:TT], ALU.mult, [xc, gg[c]], [self.yFc[6 + c]])
            self.free(r_, i_, a_, xc, gg[c])
        self.ck(6)
        W, cw = self.wload((l, 'in', 3))
        sz = [self.get('F') for _ in range(NST)]
        for st in range(NST):
            ps = self.nps()
            for kc in range(8):
                self.mm(ps[:, :], hk(kc, st * 128, (st + 1) * 128), W[:, kc * cw: kc * cw + 512], kc == 0, kc == 7, [self.hc[kc], W], [ps])
            self.act(sz[st][:, 0:TT], ps[:, :], AF.Silu, [ps], [sz[st]])
        self.ck(7)
        xbcF = []
        for slab in (4, 5):
            W, cw = self.wload((l, 'in', slab))
            for m in range(4):
                cidx = (slab - 4) * 4 + m
                ps = self.nps()
                for kc in range(8):
                    self.mm(ps[:, :], W[:, kc * cw + m * 128: kc * cw + (m + 1) * 128], hk(kc), kc == 0, kc == 7, [W, self.hc[kc]], [ps])
                raw = self.get('F')
                halo = self.shalo[l]
                self.cp('pool', raw[:, 0:3], halo[:, cidx * 3:(cidx + 1) * 3], [halo], [raw])
                self.cp('act', raw[:, 3:515], ps[:, :], [ps], [raw])
                acc = self.get('F')
                scw = lambda k: self.P(l, 'scw', cidx * 4 + k, cidx * 4 + k + 1)
                self.act(acc[:, 0:TT], raw[:, 3:515], AF.Identity, [raw, par], [acc], scale=scw(3), bias=self.P(l, 'scb', cidx, cidx + 1))
                for k in (2, 1, 0):
                    self.stt(acc[:, 0:TT], raw[:, k:k + TT], scw(k), acc[:, 0:TT], ALU.mult, ALU.add, [raw, par, acc], [acc])
                self.cp('pool', halo[:, cidx * 3:(cidx + 1) * 3], raw[:, 512:515], [raw], [halo])
                self.free(raw)
                o = self.get('H')
                self.act(o[:, :], acc[:, 0:TT], AF.Silu, [acc], [o])
                self.free(acc)
                xbcF.append(o)
        xsF, BF, CF = xbcF[0:4], xbcF[4:6], xbcF[6:8]
        self.ck(8)
        S, Sb = self.S[l], self.Sb[l]

        def ssd_A(st):
            sc = slice(st * 128, (st + 1) * 128)
            xs_TM = self.get('H')
            pt = self.nps()
            ptb = pt[:, :].bitcast(BF16)
            for c in range(4):
                self.tr(ptb[:, c * 128:(c + 1) * 128], xsF[c][:, sc], self.ident_b[:], [xsF[c], self.ident_b], [pt], inc=(c == 3))
            self.cp('act', xs_TM[:, :], ptb[:, 0:512], [pt], [xs_TM])
            B_TM = self.get('H')
            pt = self.nps()
            ptb = pt[:, :].bitcast(BF16)
            for g in range(2):
                self.tr(ptb[:, g * 128:(g + 1) * 128], BF[g][:, sc], self.ident_b[:], [BF[g], self.ident_b], [pt], inc=(g == 1))
            self.cp('act', B_TM[:, 0:256], ptb[:, 0:256], [pt], [B_TM])
            pcb = self.nps()
            for g in range(2):
                self.mm(pcb[:, g * 128:(g + 1) * 128], BF[g][:, sc], CF[g][:, sc], True, True, [BF[g], CF[g]], [pcb], inc=(g == 1))
            CBm = [self.get('L') for _ in range(2)]
            for g in range(2):
                self.tt(CBm[g][:, :], pcb[:, g * 128:(g + 1) * 128], self.U[:, :], ALU.mult, [pcb, self.U], [CBm[g]])
            MT = []
            Lrs = []
            pqs = []
            for hq in range(2):
                pq = self.nps()
                for j in range(4):
                    hd = hq * 4 + j
                    col = st * 8 + hd
                    self.mm(pq[:, j * 128:(j + 1) * 128], SdA[:, col:col + 1].to_broadcast([128, 128]), self.U[:], True, True,
                            [SdA, self.U], [pq], inc=(j == 3))
                pqs.append(pq)
            for hd in range(8):
                col = st * 8 + hd
                pq, j = pqs[hd // 4], hd % 4
                Lr = self.get('L')
                self.ts(Lr[:, :], pq[:, j * 128:(j + 1) * 128], Scum[:, col:col + 1], 0.0, ALU.subtract, ALU.min, [pq, Scum], [Lr])
                Lrs.append(Lr)
            for hd in range(8):
                col = st * 8 + hd
                self.act(Lrs[hd][:, :], Lrs[hd][:, :], AF.Exp, [Lrs[hd], Sln], [Lrs[hd]], bias=Sln[:, col:col + 1])
            for hd in range(8):
                m_ = self.get('M')
                self.tt(m_[:, :], Lrs[hd][:, :], CBm[hd // 4][:, :], ALU.mult, [Lrs[hd], CBm[hd // 4]], [m_])
                MT.append(m_)
            self.free(*Lrs)
            self.free(*CBm)
            xw = self.get('H')
            self.tt(xw[:, :].rearrange("p (h d) -> p h d", h=8), xs_TM[:, :].rearrange("p (h d) -> p h d", h=8),
                    Swg[:, st * 8:(st + 1) * 8].unsqueeze(2).to_broadcast([128, 8, 64]), ALU.mult, [xs_TM, Swg], [xw])
            return xs_TM, B_TM, MT, xw

        def ssd_B(st, xs_TM, B_TM, MT, xw):
            sc = slice(st * 128, (st + 1) * 128)
            pY = self.nps()
            for hd in range(8):
                hs_ = slice(hd * 64, (hd + 1) * 64)
                self.mm(pY[:, hs_], MT[hd][:, :], xs_TM[:, hs_], True, False, [MT[hd], xs_TM], [pY], inc=False)
                self.mm(pY[:, hs_], self.DI[l][:, hd * 128:(hd + 1) * 128], xs_TM[:, hs_], False, True, [self.DI[l], xs_TM], [pY], inc=(hd == 7))
            self.free(*MT)
            pO = self.nps()
            for g in range(2):
                self.mm(pO[:, g * 256:(g + 1) * 256], CF[g][:, sc], Sb[:, g * 256:(g + 1) * 256], True, True, [CF[g], Sb], [pO], inc=(g == 1))
            pS = self.nps()
            for g in range(2):
                self.mm(pS[:, g * 256:(g + 1) * 256], B_TM[:, g * 128:(g + 1) * 128], xw[:, g * 256:(g + 1) * 256], True, True, [B_TM, xw], [pS], inc=(g == 1))
            self.free(xw, B_TM, xs_TM)
            S3 = S[:, :].rearrange("p (h d) -> p h d", h=8)
            self.tt(S3, S3, Sdec[:, st * 8:(st + 1) * 8].unsqueeze(2).to_broadcast([128, 8, 64]), ALU.mult, [S, Sdec], [S])
            self.tt(S[:, :], S[:, :], pS[:, :], ALU.add, [S, pS], [S])
            self.cp('act', Sb[:, :], S[:, :], [S], [Sb])
            Y = self.get('F')
            self.tt(Y[:, 0:TT].rearrange("p (h d) -> p h d", h=8), pO[:, :].rearrange("p (h d) -> p h d", h=8),
                    Secum[:, st * 8:(st + 1) * 8].unsqueeze(2).to_broadcast([128, 8, 64]), ALU.mult, [pO, Secum], [Y])
            self.tt(Y[:, 0:TT], Y[:, 0:TT], pY[:, :], ALU.add, [Y, pY], [Y])
            self.tt(Y[:, 0:TT], Y[:, 0:TT], sz[st][:, 0:TT], ALU.mult, [Y, sz[st]], [Y])
            junk = self.get('F')
            ss = self.get('S')
            for g in range(2):
                self.fw.op('act', lambda a, junk=junk, Y=Y, ss=ss, g=g: a.activation(out=junk[:, g * 256:(g + 1) * 256], in_=Y[:, g * 256:(g + 1) * 256],
                                                                                  func=AF.Square, accum_out=ss[:, g:g + 1]), [Y], [junk, ss])
            self.free(junk)
            self.act(ss[:, 0:2], ss[:, 0:2], AF.Ln, [ss], [ss], scale=1.0 / 256, bias=self.epsT[:, 0:1])
            self.act(ss[:, 0:2], ss[:, 0:2], AF.Exp, [ss], [ss], scale=-0.5)
            yb = self.get('H')
            o_, n_ = POFF['snw']
            for g in range(2):
                self.stt(yb[:, g * 256:(g + 1) * 256], Y[:, g * 256:(g + 1) * 256], ss[:, g:g + 1], self.par[l][:, o_ + g * 256: o_ + (g + 1) * 256],
                         ALU.mult, ALU.mult, [Y, ss, par], [yb])
            self.free(Y, ss)
            pt = self.nps()
            ptb = pt[:, :].bitcast(BF16)
            for c in range(4):
                self.tr(ptb[:, c * 128:(c + 1) * 128], yb[:, c * 128:(c + 1) * 128], self.ident_b[:], [yb, self.ident_b], [pt], inc=(c == 3))
            self.free(yb)
            dst = self.yF[:, 2 * TT:6 * TT].rearrange("p (c t) -> p c t", c=4)[:, :, sc]
            self.cp('act', dst, ptb[:, 0:512].rearrange("p (c t) -> p c t", c=4), [pt], self.yFc[2:6])

        stA = ssd_A(0)
        for st in range(NST):
            nxt = ssd_A(st + 1) if st + 1 < NST else None
            ssd_B(st, *stA)
            stA = nxt
        self.free(*xbcF)
        self.free(*sz)
        self.free(Sdt, SdA, Sln, Scum, Secum, Sdec, Swg)
        self.dump('yF', self.yF, self.yF[:, :])
        self.ck(9)
        for m in range(8):
            if m % 4 == 0:
                W, cw = self.wload((l, 'out', m // 4))
            ps = self.nps()
            for kc in range(8):
                self.mm(ps[:, :], W[:, kc * cw + (m % 4) * 128: kc * cw + (m % 4 + 1) * 128], self.yF[:, kc * TT:(kc + 1) * TT], kc == 0, kc == 7,
                        [W, self.yFc[kc]], [ps])
            self.tt(self.xF[:, m * TT:(m + 1) * TT], ps[:, :], self.xF[:, m * TT:(m + 1) * TT], ALU.add, [ps, self.xFc[m]], [self.xFc[m]])
        self.dump('x1', self.xF, self.xF[:, :])
        self.ck(10)
        self.norm_to_h(l, 'n2w')
        hid = []
        pend = []
        fh = self.fhalo[l][ti % 2]
        fhn = self.fhalo[l][(ti + 1) % 2]
        for s in range(6):
            Wu, cwu = self.wload((l, 'up', 2 * s))
            Wv, cwv = self.wload((l, 'up', 2 * s + 1))
            for jj in range(cwu // 128):
                j = 4 * s + jj
                accs = []
                for (Wx, cwx, ci) in ((Wu, cwu, j), (Wv, cwv, 22 + j)):
                    ps = self.nps()
                    for kc in range(8):
                        self.mm(ps[:, :], Wx[:, kc * cwx + jj * 128: kc * cwx + (jj + 1) * 128], hk(kc), kc == 0, kc == 7, [Wx, self.hc[kc]], [ps])
                    acc = self.get('F')
                    fcw = lambda k, ci=ci: self.P(l, 'fcw', ci * 3 + k, ci * 3 + k + 1)
                    self.act(acc[:, 0:TT], ps[:, :], AF.Identity, [ps, par], [acc], scale=fcw(2), bias=self.P(l, 'fcb', ci, ci + 1))
                    self.cp('act', fhn[:, ci * 2: ci * 2 + 2], ps[:, TT - 2:TT], [ps], [self.fhc[l][(ti + 1) % 2][ci]])
                    self.stt(acc[:, 1:TT], ps[:, 0:TT - 1], fcw(1), acc[:, 1:TT], ALU.mult, ALU.add, [ps, par, acc], [acc])
                    self.stt(acc[:, 2:TT], ps[:, 0:TT - 2], fcw(0), acc[:, 2:TT], ALU.mult, ALU.add, [ps, par, acc], [acc])
                    hl = fh[:, ci * 2: ci * 2 + 2]
                    self.stt(acc[:, 0:1], fh[:, ci * 2 + 1: ci * 2 + 2], fcw(1), acc[:, 0:1], ALU.mult, ALU.add, [self.fhc[l][ti % 2][ci], par, acc], [acc])
                    self.stt(acc[:, 0:2], hl, fcw(0), acc[:, 0:2], ALU.mult, ALU.add, [self.fhc[l][ti % 2][ci], par, acc], [acc])
                    accs.append(acc)
                pend.append(tuple(accs))
                if len(pend) > 1:
                    self.ffn_fin(pend.pop(0), hid)
        while pend:
            self.ffn_fin(pend.pop(0), hid)
        self.ck(11)
        for m in range(8):
            W, cw = self.wload((l, 'down', m))
            ps = self.nps()
            for kc in range(22):
                self.mm(ps[:, :], W[:, kc * cw: (kc + 1) * cw], hid[kc][:, :], kc == 0, kc == 21, [W, hid[kc]], [ps])
            self.tt(self.xF[:, m * TT:(m + 1) * TT], ps[:, :], self.xF[:, m * TT:(m + 1) * TT], ALU.add, [ps, self.xFc[m]], [self.xFc[m]])
        self.free(*hid)
        self.dump('x2', self.xF, self.xF[:, :])


def _mk_eps(b):
    b.epsT = b.fw.sb([128, 1], F32)
    b.memset('pool', b.epsT, b.epsT[:], EPS)


_orig_alloc = Builder.alloc_all


def _alloc_all(self, nslots):
    _orig_alloc(self, nslots)
    _mk_eps(self)


Builder.alloc_all = _alloc_all


def make_in_maps(inputs, nseq, L, layers, ncores):
    consts = const_tables(L)
    maps = []
    shared = {}
    for l in layers:
        shared["w_in%d" % l] = np.ascontiguousarray(inputs['w_in'][l])
        shared["w_out%d" % l] = np.ascontiguousarray(inputs['w_out'][l])
        shared["w_up%d" % l] = np.ascontiguousarray(inputs['ffn_w_up'][l])
        shared["w_down%d" % l] = np.ascontiguousarray(inputs['ffn_w_down'][l])
        shared["par%d" % l] = pack_params(inputs, l)
    shared["fnw"] = np.ascontiguousarray(inputs['final_norm_w'].reshape(8, 128).T)
    for k, v in consts.items():
        shared[k] = v
    return shared


N_CORES = 8


def kernel(**inputs):
    inputs = {k: np.asarray(v) for k, v in inputs.items()}
    x = inputs['x']
    Bt, L, _ = x.shape
    nseq = Bt // N_CORES
    layers = list(range(inputs['w_in'].shape[0]))
    b = Builder(nseq, L, layers, final=True)
    shared = make_in_maps(inputs, nseq, L, layers, N_CORES)
    in_maps = []
    for c in range(N_CORES):
        m = dict(shared)
        m["x"] = np.ascontiguousarray(x[c * nseq:(c + 1) * nseq])
        in_maps.append(m)
    res = run_bass_kernel_spmd(b.nc, in_maps, core_ids=list(range(N_CORES)))
    out = np.concatenate([np.asarray(r["y"]) for r in res.results], axis=0)
    return out.astype(np.float32)
```
